# Optimizing a Trainium2 kernel written in Bass

```python
import jax
import jax.numpy as jnp
from jax import lax
import numpy as np

D_MODEL = 1024
BATCH = 32
SEQ = 2048
DEPTH = 2

GRID_W = 64
CTX_LEN = 256
NORM_EPS = 1e-6
ROPE_BASE = 10000.0

GLA_HEADS = 4
GLA_DK = 64
GLA_DV = 128
GLA_LOWRANK = 16
GLA_TEMP = 16.0
GLA_CHUNK = 64
GLA_QK = GLA_HEADS * GLA_DK
GLA_V = GLA_HEADS * GLA_DV
CONV_CH = 512
CONV_WIDTH = 31
NA_HEADS = 16
NA_HEAD_DIM = 64
NA_WIDTH = NA_HEADS * NA_HEAD_DIM
NA_WIN_R = 8
NA_WIN_C = 16
N_EXPERTS = 32
TOP_K = 4
D_EXPERT = 1024
SWIGLU_LIMIT = 7.0
SWIGLU_ALPHA = 1.702

OFF_Q = 0
OFF_G = OFF_Q + GLA_QK
OFF_GLU = OFF_G + GLA_V
OFF_K = OFF_GLU + 2 * CONV_CH
OFF_V = OFF_K + GLA_QK
OFF_AF = OFF_V + GLA_V
OFF_AB = OFF_AF + GLA_LOWRANK
W_IN_A = OFF_AB + GLA_LOWRANK
MIX_A_OUT = GLA_V + CONV_CH

kernel_name = 'hybrid_gla_conformer_natten_moe_dit'


def rmsnorm(x, g):
    xf = x.astype(jnp.float32)
    y = xf * lax.rsqrt(jnp.mean(xf * xf, axis=-1, keepdims=True) + NORM_EPS)
    return (y * g.astype(jnp.float32)).astype(x.dtype)


def layernorm(x, g, b):
    xf = x.astype(jnp.float32)
    mu = jnp.mean(xf, axis=-1, keepdims=True)
    var = jnp.mean(jnp.square(xf - mu), axis=-1, keepdims=True)
    y = (xf - mu) * lax.rsqrt(var + NORM_EPS)
    return (y * g.astype(jnp.float32) + b.astype(jnp.float32)).astype(x.dtype)


def split_heads(x, n_heads):
    b, t, _ = x.shape
    return x.reshape(b, t, n_heads, -1).transpose(0, 2, 1, 3)


def rope_1d(x, pos):
    half = x.shape[-1] // 2
    inv_freq = ROPE_BASE ** (-jnp.arange(half, dtype=jnp.float32) / half)
    ang = pos[:, None] * inv_freq[None, :]
    cos = jnp.cos(ang).astype(x.dtype)
    sin = jnp.sin(ang).astype(x.dtype)
    x1, x2 = x[..., :half], x[..., half:]
    return jnp.concatenate([x1 * cos - x2 * sin, x1 * sin + x2 * cos], axis=-1)


def axial_rope(x, row_pos, col_pos):
    half = x.shape[-1] // 2
    return jnp.concatenate([rope_1d(x[..., :half], row_pos), rope_1d(x[..., half:], col_pos)], axis=-1)


def flip_time(x):
    return x[:, :, ::-1]


def gla_log_decay(a_lr, w, b):
    z = (a_lr @ w + b).astype(jnp.float32)
    return split_heads(jax.nn.log_sigmoid(z) / GLA_TEMP, GLA_HEADS)


def gla_states(k, v, log_a, s0):
    b, h, t, _ = k.shape
    n = t // GLA_CHUNK
    kc = k.reshape(b, h, n, GLA_CHUNK, GLA_DK)
    vc = v.reshape(b, h, n, GLA_CHUNK, GLA_DV)
    bc = jnp.cumsum(log_a.reshape(b, h, n, GLA_CHUNK, GLA_DK), axis=3)
    b_last = bc[:, :, :, -1]
    kv = jnp.einsum('bhncd,bhnce->bhnde', kc * jnp.exp(b_last[:, :, :, None] - bc), vc)

    def step(s, inp):
        decay, upd = inp
        return decay[..., None] * s + upd, s

    s_final, s_before = lax.scan(step, s0, (jnp.moveaxis(jnp.exp(b_last), 2, 0), jnp.moveaxis(kv, 2, 0)))
    return jnp.moveaxis(s_before, 0, 2), s_final, bc


def gla_outputs(q, k, v, bc, s_before):
    b, h, t, _ = q.shape
    n = t // GLA_CHUNK
    qc = q.reshape(b, h, n, GLA_CHUNK, GLA_DK)
    kc = k.reshape(b, h, n, GLA_CHUNK, GLA_DK)
    vc = v.reshape(b, h, n, GLA_CHUNK, GLA_DV)
    q_t = qc * jnp.exp(bc)
    k_t = kc * jnp.exp(-bc)
    mask = jnp.tril(jnp.ones((GLA_CHUNK, GLA_CHUNK), dtype=bool))
    att = jnp.where(mask, jnp.einsum('bhncd,bhnsd->bhncs', q_t, k_t), 0.0)
    o = jnp.einsum('bhncs,bhnse->bhnce', att, vc) + jnp.einsum('bhncd,bhnde->bhnce', q_t, s_before)
    return o.reshape(b, h, t, GLA_DV)


def gla_scan(q, k, v, log_a, s0):
    s_before, s_final, bc = gla_states(k, v, log_a, s0)
    return gla_outputs(q, k, v, bc, s_before), s_final


def gla_bidir(q, k, v, la_f, la_b, s0_f, s0_b):
    o_f, s_f = gla_scan(q, k, v, la_f, s0_f)
    o_b, s_b = gla_scan(flip_time(q), flip_time(k), flip_time(v), flip_time(la_b), s0_b)
    return o_f + flip_time(o_b), s_f, s_b


def gla_post(o, g, norm_g):
    b, h, t, dv = o.shape
    of = o.astype(jnp.float32).transpose(0, 2, 1, 3)
    of = of * lax.rsqrt(jnp.mean(of * of, axis=-1, keepdims=True) + NORM_EPS)
    of = of * norm_g.astype(jnp.float32).reshape(h, dv)
    return of.reshape(b, t, h * dv).astype(g.dtype) * jax.nn.silu(g)


def conformer_conv(u2, conv_w, conv_b, ln_g, ln_b):
    a, gt = jnp.split(u2, 2, axis=-1)
    u = a * jax.nn.sigmoid(gt)
    y = lax.conv_general_dilated(
        u, conv_w[:, None, :].astype(u.dtype), window_strides=(1,),
        padding=[(CONV_WIDTH // 2, CONV_WIDTH // 2)],
        dimension_numbers=('NWC', 'WIO', 'NWC'), feature_group_count=CONV_CH)
    return jax.nn.silu(layernorm(y + conv_b, ln_g, ln_b))


def mixer_gla_conv(h_lat, h_ctx, w_in, wa_f, ba_f, wa_b, ba_b, norm_g, conv_w, conv_b, ln_g, ln_b, w_out, ctx_out):
    b, s, _ = h_lat.shape
    t = jnp.arange(s)
    row_pos = (t // GRID_W).astype(jnp.float32)
    col_pos = (t % GRID_W).astype(jnp.float32)
    q_scale = GLA_DK ** -0.5

    def sl(p, off, n, base):
        return p[..., off - base:off - base + n]

    def kv_decay(p, base):
        k = split_heads(sl(p, OFF_K, GLA_QK, base), GLA_HEADS)
        v = split_heads(sl(p, OFF_V, GLA_V, base), GLA_HEADS)
        la_f = gla_log_decay(sl(p, OFF_AF, GLA_LOWRANK, base), wa_f, ba_f)
        la_b = gla_log_decay(sl(p, OFF_AB, GLA_LOWRANK, base), wa_b, ba_b)
        return k, v, la_f, la_b

    def merge(o, p):
        y_gla = gla_post(o, sl(p, OFF_G, GLA_V, 0), norm_g)
        y_conv = conformer_conv(sl(p, OFF_GLU, 2 * CONV_CH, 0), conv_w, conv_b, ln_g, ln_b)
        return jnp.concatenate([y_gla, y_conv], axis=-1) @ w_out

    zero = jnp.zeros((h_ctx.shape[0], GLA_HEADS, GLA_DK, GLA_DV), jnp.float32)
    if ctx_out:
        p_c = h_ctx @ w_in
        k_c, v_c, laf_c, lab_c = kv_decay(p_c, 0)
        q_c = split_heads(sl(p_c, OFF_Q, GLA_QK, 0), GLA_HEADS) * q_scale
        o_c, s_f, s_b = gla_bidir(q_c, k_c, v_c, laf_c, lab_c, zero, zero)
        y_c = merge(o_c, p_c)
    else:
        p_c = h_ctx @ w_in[:, OFF_K:]
        k_c, v_c, laf_c, lab_c = kv_decay(p_c, OFF_K)
        s_f = gla_states(k_c, v_c, laf_c, zero)[1]
        s_b = gla_states(flip_time(k_c), flip_time(v_c), flip_time(lab_c), zero)[1]
        y_c = None
    p_l = h_lat @ w_in
    k_l, v_l, laf_l, lab_l = kv_decay(p_l, 0)
    q_l = axial_rope(split_heads(sl(p_l, OFF_Q, GLA_QK, 0), GLA_HEADS), row_pos, col_pos) * q_scale
    k_l = axial_rope(k_l, row_pos, col_pos)
    o_l, _, _ = gla_bidir(q_l, k_l, v_l, laf_l, lab_l, s_f, s_b)
    return merge(o_l, p_l), y_c


def softmax_attn(q, k, v):
    scale = q.shape[-1] ** -0.5
    sc = jnp.einsum('bqhd,bkhd->bhqk', q, k).astype(jnp.float32) * scale
    p = jax.nn.softmax(sc, axis=-1).astype(v.dtype)
    return jnp.einsum('bhqk,bkhd->bqhd', p, v)


def neighbourhood_attention(q, k, v, k_ctx, v_ctx, rpb):
    b, s, h, dh = q.shape
    rows = s // GRID_W
    win_r = min(NA_WIN_R, rows)
    scale = dh ** -0.5
    qg = q.reshape(b, rows, GRID_W, h, dh)
    kg = k.reshape(b, rows, GRID_W, h, dh)
    vg = v.reshape(b, rows, GRID_W, h, dh)
    cols = jnp.arange(GRID_W)
    col_start = jnp.clip(cols - NA_WIN_C // 2, 0, GRID_W - NA_WIN_C)
    col_mask = (cols[None, :] >= col_start[:, None]) & (cols[None, :] < col_start[:, None] + NA_WIN_C)
    dc_idx = jnp.clip(cols[None, :] - cols[:, None] + NA_WIN_C - 1, 0, 2 * NA_WIN_C - 2)
    rpb_col = rpb.astype(jnp.float32)[:, :, dc_idx]
    n_loc = win_r * GRID_W

    def row_block(r):
        r_start = jnp.clip(r - win_r // 2, 0, rows - win_r)
        q_r = lax.dynamic_index_in_dim(qg, r, axis=1, keepdims=False)
        k_band = lax.dynamic_slice_in_dim(kg, r_start, win_r, axis=1)
        v_band = lax.dynamic_slice_in_dim(vg, r_start, win_r, axis=1)
        dr_idx = r_start + jnp.arange(win_r) - r + NA_WIN_R - 1
        bias = jnp.take(rpb_col, dr_idx, axis=1).transpose(0, 2, 1, 3)
        bias = jnp.where(col_mask[:, None, :], bias, -jnp.inf)
        s_loc = jnp.einsum('bqhd,bjkhd->bhqjk', q_r, k_band).astype(jnp.float32) * scale + bias
        s_ctx = jnp.einsum('bqhd,bmhd->bhqm', q_r, k_ctx).astype(jnp.float32) * scale
        p = jax.nn.softmax(jnp.concatenate([s_loc.reshape(b, h, GRID_W, n_loc), s_ctx], axis=-1), axis=-1)
        p = p.astype(v.dtype)
        o = jnp.einsum('bhqjk,bjkhd->bqhd', p[..., :n_loc].reshape(b, h, GRID_W, win_r, GRID_W), v_band)
        return o + jnp.einsum('bhqm,bmhd->bqhd', p[..., n_loc:], v_ctx)

    out = lax.map(row_block, jnp.arange(rows))
    return out.transpose(1, 0, 2, 3, 4).reshape(b, s, h * dh)


def mixer_na(h_lat, h_ctx, w_qkv, rpb, w_out, ctx_out):
    b, s, _ = h_lat.shape
    l = h_ctx.shape[1]
    kv_c = (h_ctx @ w_qkv[:, NA_WIDTH:]).reshape(b, l, 2, NA_HEADS, NA_HEAD_DIM)
    k_c, v_c = kv_c[:, :, 0], kv_c[:, :, 1]
    qkv = (h_lat @ w_qkv).reshape(b, s, 3, NA_HEADS, NA_HEAD_DIM)
    o_l = neighbourhood_attention(qkv[:, :, 0], qkv[:, :, 1], qkv[:, :, 2], k_c, v_c, rpb)
    y_l = o_l @ w_out
    if ctx_out:
        q_c = (h_ctx @ w_qkv[:, :NA_WIDTH]).reshape(b, l, NA_HEADS, NA_HEAD_DIM)
        y_c = softmax_attn(q_c, k_c, v_c).reshape(b, l, NA_WIDTH) @ w_out
    else:
        y_c = None
    return y_l, y_c


def moe(h, w_r, b_r, w_gu, b_gu, w_down, b_down):
    logits = (h @ w_r + b_r).astype(jnp.float32)
    top_v, top_i = lax.top_k(logits, TOP_K)
    w = jax.nn.softmax(top_v, axis=-1)
    gates = jnp.einsum('nk,nke->ne', w, jax.nn.one_hot(top_i, N_EXPERTS, dtype=jnp.float32)).astype(h.dtype)
    out = jnp.zeros_like(h)
    for e in range(N_EXPERTS):
        gu = h @ w_gu[e] + b_gu[e]
        gt = jnp.minimum(gu[:, :D_EXPERT], SWIGLU_LIMIT)
        up = jnp.clip(gu[:, D_EXPERT:], -SWIGLU_LIMIT, SWIGLU_LIMIT)
        act = (up + 1.0) * gt * jax.nn.sigmoid(SWIGLU_ALPHA * gt)
        out = out + gates[:, e:e + 1] * (act @ w_down[e] + b_down[e])
    return out


def setup_inputs(seed: int = 0) -> dict:
    key = jax.random.key(seed)
    ks = iter(jax.random.split(key, 40))
    d = D_MODEL
    n_even = (DEPTH + 1) // 2
    n_odd = DEPTH // 2

    def nrm(shape, scale):
        return jax.random.normal(next(ks), shape, jnp.float32) * scale

    def gain(shape):
        return 1.0 + nrm(shape, 0.05)

    return {
        'x': nrm((BATCH, SEQ, d), 1.0),
        'c': nrm((BATCH, d), 1.0),
        'ctx': nrm((BATCH, CTX_LEN, d), 1.0),
        'c_ctx': nrm((d,), 1.0),
        'ada_w': nrm((DEPTH, d, 6 * d), 0.02),
        'ada_b': nrm((DEPTH, 6 * d), 0.01),
        'norm1_g': gain((DEPTH, d)),
        'norm2_g': gain((DEPTH, d)),
        'gla_conv_w_in': nrm((n_even, d, W_IN_A), d ** -0.5),
        'gla_wa_fwd': nrm((n_even, GLA_LOWRANK, GLA_QK), GLA_LOWRANK ** -0.5),
        'gla_ba_fwd': nrm((n_even, GLA_QK), 0.1),
        'gla_wa_bwd': nrm((n_even, GLA_LOWRANK, GLA_QK), GLA_LOWRANK ** -0.5),
        'gla_ba_bwd': nrm((n_even, GLA_QK), 0.1),
        'gla_norm_g': gain((n_even, GLA_V)),
        'conv_dw_w': nrm((n_even, CONV_WIDTH, CONV_CH), CONV_WIDTH ** -0.5),
        'conv_dw_b': nrm((n_even, CONV_CH), 0.01),
        'conv_ln_g': gain((n_even, CONV_CH)),
        'conv_ln_b': nrm((n_even, CONV_CH), 0.01),
        'gla_conv_w_out': nrm((n_even, MIX_A_OUT, d), MIX_A_OUT ** -0.5),
        'na_w_qkv': nrm((n_odd, d, 3 * NA_WIDTH), d ** -0.5),
        'na_rpb': nrm((n_odd, NA_HEADS, 2 * NA_WIN_R - 1, 2 * NA_WIN_C - 1), 0.1),
        'na_w_out': nrm((n_odd, NA_WIDTH, d), NA_WIDTH ** -0.5),
        'router_w': nrm((DEPTH, d, N_EXPERTS), d ** -0.5),
        'router_b': nrm((DEPTH, N_EXPERTS), 0.01),
        'expert_w_gu': nrm((DEPTH, N_EXPERTS, d, 2 * D_EXPERT), d ** -0.5),
        'expert_b_gu': nrm((DEPTH, N_EXPERTS, 2 * D_EXPERT), 0.01),
        'expert_w_down': nrm((DEPTH, N_EXPERTS, D_EXPERT, d), D_EXPERT ** -0.5),
        'expert_b_down': nrm((DEPTH, N_EXPERTS, d), 0.01),
        'final_norm_g': gain((d,)),
    }


def reference(x, c, ctx, c_ctx, ada_w, ada_b, norm1_g, norm2_g, gla_conv_w_in, gla_wa_fwd, gla_ba_fwd,
              gla_wa_bwd, gla_ba_bwd, gla_norm_g, conv_dw_w, conv_dw_b, conv_ln_g, conv_ln_b, gla_conv_w_out,
              na_w_qkv, na_rpb, na_w_out, router_w, router_b, expert_w_gu, expert_b_gu, expert_w_down,
              expert_b_down, final_norm_g):
    d = x.shape[-1]
    x_lat, x_ctx = x, ctx
    sc = jax.nn.silu(c)
    scc = jax.nn.silu(c_ctx)
    for i in range(DEPTH):
        last = i == DEPTH - 1
        j = i // 2
        mod_l = (sc @ ada_w[i] + ada_b[i])[:, None, :]
        sh1, s1, g1, sh2, s2, g2 = jnp.split(mod_l, 6, axis=-1)
        n_mod = 2 if last else 6
        mc = jnp.split(scc @ ada_w[i][:, :n_mod * d] + ada_b[i][:n_mod * d], n_mod)
        h_l = rmsnorm(x_lat, norm1_g[i]) * (1.0 + s1) + sh1
        h_c = rmsnorm(x_ctx, norm1_g[i]) * (1.0 + mc[1]) + mc[0]
        if i % 2 == 0:
            y_l, y_c = mixer_gla_conv(h_l, h_c, gla_conv_w_in[j], gla_wa_fwd[j], gla_ba_fwd[j], gla_wa_bwd[j],
                                      gla_ba_bwd[j], gla_norm_g[j], conv_dw_w[j], conv_dw_b[j], conv_ln_g[j],
                                      conv_ln_b[j], gla_conv_w_out[j], not last)
        else:
            y_l, y_c = mixer_na(h_l, h_c, na_w_qkv[j], na_rpb[j], na_w_out[j], not last)
        x_lat = x_lat + g1 * y_l
        h2_l = rmsnorm(x_lat, norm2_g[i]) * (1.0 + s2) + sh2
        if last:
            f = moe(h2_l.reshape(-1, d), router_w[i], router_b[i], expert_w_gu[i], expert_b_gu[i],
                    expert_w_down[i], expert_b_down[i])
            x_lat = x_lat + g2 * f.reshape(x_lat.shape)
        else:
            x_ctx = x_ctx + mc[2] * y_c
            h2_c = rmsnorm(x_ctx, norm2_g[i]) * (1.0 + mc[4]) + mc[3]
            n_lat = h2_l.shape[0] * h2_l.shape[1]
            tokens = jnp.concatenate([h2_l.reshape(-1, d), h2_c.reshape(-1, d)], axis=0)
            f = moe(tokens, router_w[i], router_b[i], expert_w_gu[i], expert_b_gu[i],
                    expert_w_down[i], expert_b_down[i])
            x_lat = x_lat + g2 * f[:n_lat].reshape(x_lat.shape)
            x_ctx = x_ctx + mc[5] * f[n_lat:].reshape(x_ctx.shape)
    return rmsnorm(x_lat, final_norm_g)
```

```python
import numpy as np
from contextlib import ExitStack
import concourse.bass as bass
import concourse.mybir as mybir
from concourse.bass_utils import run_bass_kernel_spmd

F32 = mybir.dt.float32
BF16 = mybir.dt.bfloat16
AF = mybir.ActivationFunctionType
ALU = mybir.AluOpType
AX = mybir.AxisListType

D = 1024
SEQ = 2048
CTX = 256
NCORES = 8
NS = 4
EPS = 1e-6
NE = 32
W_IN_A = 2592
OFF_Q, OFF_G, OFF_GLU, OFF_K, OFF_V, OFF_AF, OFF_AB = 0, 256, 768, 1792, 2048, 2560, 2576


class Trk:
    __slots__ = ("w", "r")

    def __init__(self):
        self.w = None
        self.r = {}


class Eng:
    def __init__(self, name, be, sem, is_dma_only=False):
        self.name = name
        self.be = be
        self.sem = sem
        self.count = 0
        self.waited = {}


class Prog:
    def __init__(self, nc, es):
        self.nc = nc
        self.es = es
        self.E = {}
        for name, be in (("pe", nc.tensor), ("act", nc.scalar), ("dve", nc.vector),
                         ("pool", nc.gpsimd), ("sp", nc.sync)):
            sem = es.enter_context(nc.semaphore("s_" + name))
            self.E[name] = Eng(name, be, sem)
        self.dsem = {}
        self.semobj = {}
        for e in self.E.values():
            self.semobj[id(e.sem)] = e.sem
        self.ninstr = 0

    def _waits(self, e, r, w, is_dma):
        deps = {}

        def add(ev):
            k, v, en = ev
            if deps.get(k, (0, None))[0] < v:
                deps[k] = (v, en)

        for t in r:
            if t.w is not None:
                add(t.w)
        for t in w:
            if t.w is not None:
                if is_dma or t.w[2] != e.name:
                    add(t.w)
            for k, (v, en) in t.r.items():
                if is_dma or en != e.name:
                    add((k, v, en))
        for k, (v, en) in deps.items():
            if e.waited.get(k, 0) >= v:
                continue
            e.waited[k] = v
            e.be.wait_ge(self.semobj[k], v)

    def _mark(self, ev, r, w):
        for t in w:
            t.w = ev
            t.r = {}
        for t in r:
            k, v, en = ev
            if t.r.get(k, (0, None))[0] < v:
                t.r[k] = (v, en)

    def op(self, en, fn, r=(), w=()):
        e = self.E[en]
        self._waits(e, r, w, False)
        ins = fn(e.be)
        e.count += 1
        ins.then_inc(e.sem, 1)
        ev = (id(e.sem), e.count, en)
        self._mark(ev, r, w)
        self.ninstr += 1
        return ev

    def dma(self, qn, out, in_, r=(), w=(), key=None, **kw):
        e = self.E[qn]
        if key is None:
            key = "dflt_" + qn
        if key not in self.dsem:
            sem = self.es.enter_context(self.nc.semaphore("d_" + key))
            self.dsem[key] = [sem, 0]
            self.semobj[id(sem)] = sem
        self._waits(e, r, w, True)
        ent = self.dsem[key]
        ins = e.be.dma_start(out=out, in_=in_, **kw)
        ent[1] += 16
        ins.then_inc(ent[0], 16)
        ev = (id(ent[0]), ent[1], "dma_" + key)
        self._mark(ev, r, w)
        self.ninstr += 1
        return ev

    def barrier(self):
        evs = [(id(e.sem), e.count) for e in self.E.values() if e.count > 0]
        evs += [(id(s), c) for (s, c) in self.dsem.values() if c > 0]
        for e in self.E.values():
            for k, v in evs:
                if k == id(e.sem) and e.name in ("pe", "pool", "sp"):
                    continue
                if e.waited.get(k, 0) >= v:
                    continue
                e.waited[k] = v
                e.be.wait_ge(self.semobj[k], v)

    def wait_all(self, en, trks):
        e = self.E[en]
        self._waits(e, trks, (), True)


class Builder:
    def __init__(self, ns=NS, nlayers=2, debug=None, moe_experts=NE):
        self.ns = ns
        self.nlayers = nlayers
        self.debug = debug or ()
        self.moe_experts = moe_experts
        self.nc = bass.Bass("TRN2", target_bir_lowering=False)
        self.es = ExitStack()
        self.P = Prog(self.nc, self.es)
        self.cur = self.es
        self.dram = {}

    def din(self, name, shape, dt=F32):
        t = self.nc.dram_tensor(name, list(shape), dt, kind="ExternalInput").ap()
        self.dram[name] = t
        return t

    def dout(self, name, shape, dt=F32):
        t = self.nc.dram_tensor(name, list(shape), dt, kind="ExternalOutput").ap()
        self.dram[name] = t
        return t

    def sb(self, name, shape, dt=F32):
        self._nm = getattr(self, "_nm", 0) + 1
        return self.cur.enter_context(self.nc.sbuf_tensor(f"sb{self._nm}_" + name, list(shape), dt))

    def dump2d(self, dst, src, n, trks):
        P = self.P
        for c0 in range(0, n, 1024):
            w_ = min(1024, n - c0)
            P.op("dve", lambda e, c0=c0, w_=w_: e.tensor_copy(out=self.dbg_stage[:, 0:w_], in_=src[:, c0:c0 + w_]),
                 r=list(trks), w=[self.dbg_stage_t])
            P.dma("sp", dst[:, c0:c0 + w_], self.dbg_stage[:, 0:w_], r=[self.dbg_stage_t], w=[Trk()], key="dbg")

    def phase(self):
        b = self

        class _Ph:
            def __enter__(s_):
                s_.prev = b.cur
                s_.st = ExitStack()
                b.cur = s_.st
                return s_

            def __exit__(s_, *a):
                b.P.barrier()
                s_.st.close()
                b.cur = s_.prev
                return False

        return _Ph()

    def build(self):
        nc, P = self.nc, self.P
        ns = self.ns
        x_d = self.din("x", [ns, SEQ, D])
        ctx_d = self.din("ctx", [ns, CTX, D])
        cT_d = self.din("cT", [128, 8, 5])
        ada_w_d = self.din("ada_w", [2, D, 6 * D])
        ada_bT_d = self.din("ada_bT", [2, 128, 48])
        n1g_d = self.din("n1gT", [2, 128, 8])
        n2g_d = self.din("n2gT", [2, 128, 8])
        fng_d = self.din("fng", [D])
        ident_d = self.din("ident", [128, 128])
        out_d = self.dout("out", [ns, SEQ, D])
        router_w_d = self.din("router_w", [2, D, NE])
        router_b_d = self.din("router_b", [2, NE])
        bguT_d = self.din("bguT", [2, 128, NE, 16])
        expert_b_down_d = self.din("expert_b_down", [2, NE, D])
        expert_w_gu_d = self.din("expert_w_gu", [2, NE, D, 2 * D])
        expert_w_down_d = self.din("expert_w_down", [2, NE, D, D])
        self.dbg_layer = 0
        na_w_qkv_d = self.din("na_w_qkv", [D, 3 * D])
        gla_w_in_d = self.din("gla_w_in", [D, W_IN_A])
        gla_w_out_d = self.din("gla_w_out", [D, D])
        gconst_d = self.din("gconst", [128, 772])
        convp_d = self.din("convp", [128, 4, 34])
        wabd_d = self.din("wabd", [33, 512])
        gla_norm_g_d = self.din("gla_norm_g", [512])
        ropeC_d = self.din("ropeC", [128, 18, 64])
        ropeS_d = self.din("ropeS", [128, 18, 64])
        na_w_out_d = self.din("na_w_out", [D, D])
        na_bt_d = self.din("na_bt", [16, 128, 5, 640])
        if "gates" in self.debug:
            dbg_gates = self.dout("dbg_gates", [128, 18 * NE])
        if "xs" in self.debug:
            dbg_xs = self.dout("dbg_xs", [128, 18, D])
        if "hT" in self.debug:
            dbg_hT = self.dout("dbg_hT", [128, 8, 2304])
        if "mod" in self.debug:
            dbg_mod = self.dout("dbg_mod", [128, 2, 48, 5])

        xs = self.sb("xs", [128, 18, D])
        xs_t = [Trk() for _ in range(18)]
        hT = self.sb("hT", [128, 8, 2304], BF16)
        hT_t = [Trk() for _ in range(5)]
        ps = self.es.enter_context(nc.psum_tensor("ps", [128, 8, 512], F32))
        ps_t = [Trk() for _ in range(8)]
        identf = self.sb("identf", [128, 128])
        identb = self.sb("identb", [128, 128], BF16)
        cst_t = Trk()
        epsc = self.sb("epsc", [128, 1])
        c11 = self.sb("c11", [128, 1])
        onesf = self.sb("onesf", [128, 128])
        onesm = self.sb("onesm", [128, 128])
        modT = self.sb("modT", [128, 2, 48, 5])
        mod_t = Trk()
        A1 = self.sb("A1", [128, 2, 2, 8, 5])
        A_t = Trk()
        ng = self.sb("ng", [128, 2, 2, 8])
        self.bc_diag = [self.sb(f"bc_diag{i}", [128, 128]) for i in range(2)]
        self.bc_diag_t = [Trk(), Trk()]
        self.bc_it = 0
        if self.debug:
            self.dbg_stage = self.sb("dbg_stage", [128, 1024])
            self.dbg_stage_t = Trk()
        self.__dict__.update(locals())

        P.dma("sp", identf[:], ident_d, w=[cst_t], key="c0")
        P.op("dve", lambda e: e.tensor_copy(out=identb[:], in_=identf[:]), r=[cst_t], w=[cst_t])
        P.op("dve", lambda e: e.memset(epsc[:], EPS), w=[cst_t])
        P.op("dve", lambda e: e.memset(c11[:], 7.0 * 1.702), w=[cst_t])
        P.op("dve", lambda e: e.memset(onesf[:], 1.0), w=[cst_t])
        P.op("dve", lambda e: e.memset(onesm[:], 1.0 / 512), w=[cst_t])
        ng_t = Trk()
        for l in range(2):
            P.dma("sp", ng[:, l, 0, :], n1g_d[l], w=[ng_t], key="c0")
            P.dma("sp", ng[:, l, 1, :], n2g_d[l], w=[ng_t], key="c0")
        self.ng_t = ng_t

        with self.phase():
            self.compute_mod()
        if "mod" in self.debug:
            P.dma("sp", dbg_mod, modT[:], r=[mod_t], w=[Trk()], key="dbg")

        self.out_trks = []
        for b in range(ns):
            if "nosample" in self.debug:
                break
            self.sample(b)

        P.wait_all("sp", self.out_trks)
        return nc

    def compute_mod(self):
        nc, P = self.nc, self.P
        cT = self.sb("cT", [128, 8, 5])
        scT = self.sb("scT", [128, 8, 5])
        abT = self.sb("abT", [128, 2, 48])
        c_t = Trk()
        P.dma("sp", cT[:], self.cT_d, w=[c_t], key="c0")
        P.dma("sp", abT[:, 0, :], self.ada_bT_d[0], w=[c_t], key="c0")
        P.dma("sp", abT[:, 1, :], self.ada_bT_d[1], w=[c_t], key="c0")
        P.op("act", lambda e: e.activation(out=scT[:], in_=cT[:], func=AF.Silu), r=[c_t], w=[c_t])
        wst = [self.sb(f"adaw{i}", [128, 8, 512]) for i in range(2)]
        wst_t = [Trk(), Trk()]
        modT, mod_t, ps, ps_t = self.modT, self.mod_t, self.ps, self.ps_t
        wv = self.ada_w_d.rearrange("l (kc p) f -> l p kc f", p=128)
        it = 0
        for l in range(2):
            pbank = ps[:, l, :]
            for cb in range(12):
                buf = it % 2
                q = "sp" if it % 2 == 0 else "act"
                P.dma(q, wst[buf][:], wv[l, :, :, cb * 512:(cb + 1) * 512], w=[wst_t[buf]], key=f"adaw{buf}")
                for jj in range(4):
                    j = cb * 4 + jj
                    for kc in range(8):
                        P.op("pe", lambda e, buf=buf, kc=kc, jj=jj, j=j, pbank=pbank: e.matmul(
                            pbank[:, j * 5:(j + 1) * 5], wst[buf][:, kc, jj * 128:(jj + 1) * 128], scT[:, kc, :],
                            start=(kc == 0), stop=(kc == 7)), r=[wst_t[buf], c_t], w=[ps_t[l]])
                it += 1
            P.op("dve", lambda e, l=l, pbank=pbank: e.tensor_tensor(
                out=modT[:, l, :, :], in0=pbank[:, 0:240].rearrange("p (j b) -> p j b", b=5),
                in1=abT[:, l, :].unsqueeze(2).to_broadcast([128, 48, 5]), op=ALU.add),
                r=[ps_t[l], c_t], w=[mod_t])
        for l in range(2):
            for wh, grp in ((0, 1), (1, 4)):
                P.op("dve", lambda e, l=l, wh=wh, grp=grp: e.scalar_tensor_tensor(
                    out=self.A1[:, l, wh, :, :], in0=modT[:, l, grp * 8:(grp + 1) * 8, :], scalar=1.0,
                    in1=self.ng[:, l, wh, :].unsqueeze(2).to_broadcast([128, 8, 5]),
                    op0=ALU.add, op1=ALU.mult), r=[mod_t, self.ng_t], w=[self.A_t])

    def prenorm(self, b, l, wh, tiles):
        nc, P = self.nc, self.P
        xs, xs_t, hT, hT_t, ps, ps_t = self.xs, self.xs_t, self.hT, self.hT_t, self.ps, self.ps_t
        shgrp = 0 if wh == 0 else 3
        self.pn_ss = [self.sb(f"pn_ss{i}", [128, 2]) for i in range(2)]
        self.pn_ss_t = [Trk() for _ in range(2)]
        self.pn_xb = [self.sb(f"pn_xb{i}", [128, D], BF16) for i in range(2)]
        self.pn_xb_t = [Trk() for _ in range(2)]
        self.pn_it = 0
        for g0 in range(0, len(tiles), 4):
            grp = tiles[g0:g0 + 4]
            ng_ = len(grp)
            gi = grp[0] // 4
            psb = ps.bitcast(BF16)
            for ti, t in enumerate(grp):
                i = self.pn_it % 2
                self.pn_it += 1
                ss, ss_t, xb, xb_t = self.pn_ss[i], self.pn_ss_t[i], self.pn_xb[i], self.pn_xb_t[i]
                P.op("act", lambda e, t=t, ss=ss: e.activation(
                    out=xb[:], in_=xs[:, t, :], func=AF.Square, accum_out=ss[:, 0:1]),
                    r=[xs_t[t]], w=[xb_t, ss_t])
                if "pn1" in self.debug:
                    continue
                P.op("act", lambda e, ss=ss: e.activation(
                    out=ss[:, 1:2], in_=ss[:, 0:1], func=AF.Sqrt, scale=1.0 / D, bias=self.epsc[:, 0:1]),
                    r=[ss_t, self.cst_t], w=[ss_t])
                P.op("dve", lambda e, ss=ss: e.reciprocal(out=ss[:, 1:2], in_=ss[:, 1:2]),
                     r=[ss_t], w=[ss_t])
                if "pn2" in self.debug:
                    continue
                P.op("dve", lambda e, t=t, ss=ss, xb=xb: e.tensor_scalar(
                    out=xb[:], in0=xs[:, t, :], scalar1=ss[:, 1:2], scalar2=None, op0=ALU.mult),
                    r=[ss_t, xs_t[t]], w=[xb_t])
                if "pn3" in self.debug:
                    continue
                for kc in range(8):
                    bank = 4 + kc // 2
                    off = (kc % 2) * 512 + ti * 128
                    P.op("pe", lambda e, bank=bank, off=off, xb=xb, kc=kc: e.transpose(
                        psb[:, bank, off:off + 128], xb[:, kc * 128:(kc + 1) * 128], self.identb[:]),
                        r=[xb_t, self.cst_t], w=[ps_t[bank]])
            if "pn1" in self.debug or "pn2" in self.debug or "pn3" in self.debug or "pn4" in self.debug:
                continue
            ntok = ng_ * 128
            tok0 = grp[0] * 128
            isctx = grp[0] >= 16
            bcol = 4 if isctx else b
            for kc in range(8):
                bank = 4 + kc // 2
                off = (kc % 2) * 512
                P.op("act", lambda e, bank=bank, off=off, kc=kc, ntok=ntok, tok0=tok0, bcol=bcol: e.activation(
                    out=hT[:, kc, tok0:tok0 + ntok], in_=psb[:, bank, off:off + ntok], func=AF.Identity,
                    scale=self.A1[:, l, wh, kc, bcol:bcol + 1],
                    bias=self.modT[:, l, shgrp * 8 + kc, bcol:bcol + 1]),
                    r=[ps_t[bank], self.A_t, self.mod_t], w=[hT_t[gi]])


    def bcast_cols(self, dst, col_fn, dst_trk):
        P, ps, ps_t = self.P, self.ps, self.ps_t
        for kc in range(8):
            i = self.bc_it % 2
            self.bc_it += 1
            dg, dg_t = self.bc_diag[i], self.bc_diag_t[i]
            P.op("dve", lambda e, dg=dg, kc=kc: e.tensor_scalar(
                out=dg[:], in0=self.identf[:], scalar1=col_fn(kc), scalar2=None, op0=ALU.mult),
                r=[self.cst_t, self.mod_t], w=[dg_t])
            bank = kc // 4
            P.op("pe", lambda e, dg=dg, kc=kc, bank=bank: e.matmul(
                ps[:, bank, (kc % 4) * 128:(kc % 4 + 1) * 128], self.onesf[:], dg[:], start=True, stop=True),
                r=[dg_t, self.cst_t], w=[ps_t[bank]])
        for bank in range(2):
            P.op("act", lambda e, bank=bank: e.activation(
                out=dst[:, bank * 512:(bank + 1) * 512], in_=ps[:, bank, :], func=AF.Copy),
                r=[ps_t[bank]], w=[dst_trk])

    def moe(self, b, l, ntiles):
        nc, P = self.nc, self.P
        xs, xs_t, hT, hT_t, ps, ps_t = self.xs, self.xs_t, self.hT, self.hT_t, self.ps, self.ps_t
        modT = self.modT
        ngroups = (ntiles + 3) // 4
        gwid = [min(512, ntiles * 128 - g * 512) for g in range(ngroups)]
        wr = self.sb("wr", [128, 8, NE], BF16)
        brb = self.sb("brb", [128, NE])
        bgT = self.sb("bgT", [128, NE, 16])
        bd = self.sb("bd", [NE, D])
        tb_t = Trk()
        P.dma("pool", wr[:], self.router_w_d[l].rearrange("(kc p) e -> p kc e", p=128), w=[tb_t], key="mt0")
        P.dma("sp", brb[:], self.router_b_d[l].partition_broadcast(128), w=[tb_t], key="mt1")
        P.dma("sp", bgT[:], self.bguT_d[l], w=[tb_t], key="mt1")
        P.dma("sp", bd[:], self.expert_b_down_d[l], w=[tb_t], key="mt1")
        P.op("dve", lambda e: e.tensor_scalar(out=bgT[:, :, 0:8], in0=bgT[:, :, 0:8], scalar1=-1.0, scalar2=7.0,
                                              op0=ALU.mult, op1=ALU.add), r=[tb_t], w=[tb_t])
        P.op("dve", lambda e: e.tensor_scalar(out=bgT[:, :, 8:16], in0=bgT[:, :, 8:16], scalar1=1.0, scalar2=None,
                                              op0=ALU.add), r=[tb_t], w=[tb_t])
        gbc = self.sb("gbc", [128, 2, D])
        gbc_t = [Trk(), Trk()]
        self.bcast_cols(gbc[:, 0, :], lambda kc: modT[:, l, 40 + kc, b:b + 1], gbc_t[0])
        if ntiles > 16:
            self.bcast_cols(gbc[:, 1, :], lambda kc: modT[:, l, 40 + kc, 4:5], gbc_t[1])
        gates = self.sb("gates", [128, 18, NE])
        gates_s = self.sb("gates_s", [128, 18, NE])
        g_t = [Trk() for _ in range(18)]
        lg = [self.sb(f"lg{i}", [128, NE]) for i in range(2)]
        ex = [self.sb(f"ex{i}", [128, NE]) for i in range(2)]
        mx8 = [self.sb(f"mx{i}", [128, 12]) for i in range(2)]
        gT = [self.sb(f"gT{i}", [NE, 128]) for i in range(2)]
        tmpacc = [self.sb("tmpacc", [128, D])] * 2
        tmpacc_t = [Trk()] * 2
        r_t = [Trk(), Trk()]
        gT_t = [Trk(), Trk()]
        for t in range(ntiles):
            i = t % 2
            gi = t // 4
            bank = i
            P.op("pe", lambda e: e.engine_nop(), r=[], w=[]) if False else None
            for kc in range(8):
                P.op("pe", lambda e, kc=kc, t=t, bank=bank: e.matmul(
                    ps[:, bank, 0:NE], hT[:, kc, t * 128:(t + 1) * 128], wr[:, kc, :], start=(kc == 0), stop=(kc == 7)),
                    r=[hT_t[gi], tb_t], w=[ps_t[bank]])
            P.op("dve", lambda e, i=i, bank=bank: e.tensor_tensor(out=lg[i][:], in0=ps[:, bank, 0:NE], in1=brb[:], op=ALU.add),
                 r=[ps_t[bank], tb_t], w=[r_t[i]])
            P.op("dve", lambda e, i=i: e.max(out=mx8[i][:, 0:8], in_=lg[i][:]), r=[r_t[i]], w=[r_t[i]])
            P.op("dve", lambda e, i=i: e.tensor_scalar(out=mx8[i][:, 8:9], in0=mx8[i][:, 0:1], scalar1=-1.0, scalar2=None,
                                                       op0=ALU.mult), r=[r_t[i]], w=[r_t[i]])
            P.op("act", lambda e, i=i: e.activation(out=ex[i][:], in_=lg[i][:], func=AF.Exp, bias=mx8[i][:, 8:9], scale=1.0),
                 r=[r_t[i]], w=[r_t[i]])
            P.op("dve", lambda e, i=i: e.scalar_tensor_tensor(
                out=ex[i][:], in0=lg[i][:], scalar=mx8[i][:, 3:4], in1=ex[i][:], op0=ALU.is_ge, op1=ALU.mult,
                accum_out=mx8[i][:, 9:10]), r=[r_t[i]], w=[r_t[i]])
            P.op("dve", lambda e, i=i: e.reciprocal(out=mx8[i][:, 10:11], in_=mx8[i][:, 9:10]), r=[r_t[i]], w=[r_t[i]])
            P.op("dve", lambda e, i=i, t=t: e.tensor_scalar(out=gates[:, t, :], in0=ex[i][:], scalar1=mx8[i][:, 10:11],
                                                            scalar2=None, op0=ALU.mult), r=[r_t[i]], w=[g_t[t]])
            P.op("dve", lambda e, i=i, t=t: e.tensor_scalar(out=gates_s[:, t, :], in0=ex[i][:], scalar1=mx8[i][:, 10:11],
                                                            scalar2=1.0 / 1.702, op0=ALU.mult, op1=ALU.mult),
                 r=[r_t[i]], w=[g_t[t]])
            P.op("pe", lambda e, t=t, bank=bank: e.transpose(ps[0:NE, bank, 128:256], gates[:, t, :], self.identf[:]),
                 r=[g_t[t], self.cst_t], w=[ps_t[bank]])
            P.op("act", lambda e, i=i, bank=bank: e.activation(out=gT[i][:], in_=ps[0:NE, bank, 128:256], func=AF.Copy),
                 r=[ps_t[bank]], w=[gT_t[i]])
            for hh in range(2):
                bk = 2 + 2 * i + hh
                P.op("pe", lambda e, i=i, hh=hh, bk=bk: e.matmul(ps[:, bk, :], gT[i][:], bd[:, hh * 512:(hh + 1) * 512],
                                                               start=True, stop=True),
                     r=[gT_t[i], tb_t], w=[ps_t[bk]])
            cx = 1 if t >= 16 else 0
            P.op("dve", lambda e, i=i, cx=cx: e.tensor_tensor(
                out=tmpacc[i][:], in0=ps[:, 2 + 2 * i:4 + 2 * i, :].rearrange("p a n -> p (a n)"), in1=gbc[:, cx, :], op=ALU.mult),
                r=[ps_t[2 + 2 * i], ps_t[3 + 2 * i], gbc_t[cx]], w=[tmpacc_t[i]])
            P.op("dve", lambda e, i=i, t=t: e.tensor_tensor(out=xs[:, t, :], in0=xs[:, t, :], in1=tmpacc[i][:], op=ALU.add),
                 r=[tmpacc_t[i], xs_t[t]], w=[xs_t[t]])
        if "gates" in self.debug and b == 0 and l == self.dbg_layer:
            self.dump2d(self.dbg_gates, gates[:].rearrange("p t e -> p (t e)"), 18 * NE, g_t)

        wg = [self.sb(f"wg{i}", [128, 8, 512], BF16) for i in range(2)]
        wu = [self.sb(f"wu{i}", [128, 8, 512], BF16) for i in range(2)]
        wd = [self.sb(f"wd{i}", [128, 4, D], BF16) for i in range(2)]
        w_t = [Trk(), Trk()]
        actb = [self.sb(f"actb{i}", [128, 4, 512], BF16) for i in range(2)]
        actb_t = [Trk(), Trk()]
        rbuf = [self.sb(f"rbuf{i}", [128, 512]) for i in range(2)]
        slbuf = [self.sb(f"slbuf{i}", [128, 512], BF16) for i in range(2)]
        uabuf = [self.sb(f"uabuf{i}", [128, 512]) for i in range(2)]
        el_t = [Trk(), Trk()]
        wgu_v = self.expert_w_gu_d.rearrange("l e (kc p) f -> l e p kc f", p=128)
        wd_v = self.expert_w_down_d.rearrange("l e (kc p) f -> l e p kc f", p=128)
        nhe = self.moe_experts * 2

        def load_w(he):
            e_, hf = he // 2, he % 2
            i = he % 2
            P.dma("pool", wg[i][:], wgu_v[l, e_, :, :, hf * 512:(hf + 1) * 512], w=[w_t[i]], key=f"wg{i}")
            P.dma("pool", wu[i][:], wgu_v[l, e_, :, :, 1024 + hf * 512:1024 + (hf + 1) * 512], w=[w_t[i]], key=f"wg{i}")
            P.dma("pool", wd[i][:], wd_v[l, e_, :, hf * 4:(hf + 1) * 4, :], w=[w_t[i]], key=f"wg{i}")

        units = [(he, g) for he in range(nhe) for g in range(ngroups)]
        self._fcit = 0

        def gu_step(u, fc):
            he, g = units[u]
            e_, hf = he // 2, he % 2
            wi = he % 2
            gw = gwid[g]
            par = self._fcit % 2
            self._fcit += 1
            bg_, bu_ = 2 * par, 2 * par + 1
            for kc in range(8):
                P.op("pe", lambda e, kc=kc: e.matmul(ps[:, bg_, 0:gw], wg[wi][:, kc, fc * 128:(fc + 1) * 128],
                                                    hT[:, kc, g * 512:g * 512 + gw], start=(kc == 0), stop=(kc == 7)),
                     r=[w_t[wi], hT_t[g]], w=[ps_t[bg_]])
            for kc in range(8):
                P.op("pe", lambda e, kc=kc: e.matmul(ps[:, bu_, 0:gw], wu[wi][:, kc, fc * 128:(fc + 1) * 128],
                                                    hT[:, kc, g * 512:g * 512 + gw], start=(kc == 0), stop=(kc == 7)),
                     r=[w_t[wi], hT_t[g]], w=[ps_t[bu_]])
            cg = hf * 4 + fc
            P.op("act", lambda e: e.activation(out=rbuf[par][:, 0:gw], in_=ps[:, bg_, 0:gw], func=AF.Relu,
                                               scale=-1.0, bias=bgT[:, e_, cg:cg + 1]),
                 r=[ps_t[bg_], tb_t], w=[el_t[par]])
            P.op("act", lambda e: e.activation(out=slbuf[par][:, 0:gw], in_=rbuf[par][:, 0:gw], func=AF.Silu,
                                               scale=-1.702, bias=self.c11[:, 0:1]),
                 r=[el_t[par], self.cst_t], w=[el_t[par]])
            P.op("dve", lambda e: e.tensor_scalar(out=uabuf[par][:, 0:gw], in0=ps[:, bu_, 0:gw],
                                                  scalar1=bgT[:, e_, 8 + cg:9 + cg], scalar2=8.0, op0=ALU.add, op1=ALU.min),
                 r=[ps_t[bu_], tb_t], w=[el_t[par]])
            P.op("dve", lambda e: e.scalar_tensor_tensor(out=actb[u % 2][:, fc, 0:gw], in0=uabuf[par][:, 0:gw], scalar=-6.0,
                                                         in1=slbuf[par][:, 0:gw], op0=ALU.max, op1=ALU.mult),
                 r=[el_t[par]], w=[actb_t[u % 2]])

        self._dit = 0

        def down(u):
            he, g = units[u]
            e_, hf = he // 2, he % 2
            wi = he % 2
            nt = gwid[g] // 128
            for tt in range(nt):
                t = g * 4 + tt
                dp = self._dit % 2
                self._dit += 1
                for hh in range(2):
                    bk = 4 + 2 * dp + hh
                    for fc in range(4):
                        P.op("pe", lambda e, fc=fc, bk=bk, hh=hh: e.matmul(
                            ps[:, bk, :], actb[u % 2][:, fc, tt * 128:(tt + 1) * 128], wd[wi][:, fc, hh * 512:(hh + 1) * 512],
                            start=(fc == 0), stop=(fc == 3)), r=[actb_t[u % 2], w_t[wi]], w=[ps_t[bk]])
                cx = 1 if t >= 16 else 0
                P.op("dve", lambda e, dp=dp, cx=cx: e.tensor_tensor(
                    out=tmpacc[dp][:], in0=ps[:, 4 + 2 * dp:6 + 2 * dp, :].rearrange("p a n -> p (a n)"), in1=gbc[:, cx, :],
                    op=ALU.mult), r=[ps_t[4 + 2 * dp], ps_t[5 + 2 * dp], gbc_t[cx]], w=[tmpacc_t[dp]])
                P.op("dve", lambda e, dp=dp, t=t: e.scalar_tensor_tensor(
                    out=xs[:, t, :], in0=tmpacc[dp][:], scalar=gates_s[:, t, e_:e_ + 1], in1=xs[:, t, :],
                    op0=ALU.mult, op1=ALU.add), r=[tmpacc_t[dp], g_t[t], xs_t[t]], w=[xs_t[t]])

        load_w(0)
        if nhe > 1:
            load_w(1)
        for fc in range(4):
            gu_step(0, fc)
        for u in range(len(units)):
            he, g = units[u]
            if u + 1 < len(units):
                gu_step(u + 1, 0)
            down(u)
            if g == ngroups - 1 and he + 2 < nhe:
                load_w(he + 2)
            if u + 1 < len(units):
                for fc in range(1, 4):
                    gu_step(u + 1, fc)


    def resid_add(self, t, bank0, gbc_ap, gbc_trk, tmp, tmp_t):
        P, ps, ps_t, xs, xs_t = self.P, self.ps, self.ps_t, self.xs, self.xs_t
        P.op("dve", lambda e: e.tensor_tensor(
            out=tmp[:], in0=ps[:, bank0:bank0 + 2, :].rearrange("p a n -> p (a n)"), in1=gbc_ap, op=ALU.mult),
            r=[ps_t[bank0], ps_t[bank0 + 1], gbc_trk], w=[tmp_t])
        P.op("dve", lambda e: e.tensor_tensor(out=xs[:, t, :], in0=xs[:, t, :], in1=tmp[:], op=ALU.add),
             r=[tmp_t, xs_t[t]], w=[xs_t[t]])

    def mixer_na(self, b, l):
        nc, P = self.nc, self.P
        xs, xs_t, hT, hT_t, ps, ps_t = self.xs, self.xs_t, self.hT, self.hT_t, self.ps, self.ps_t
        psb = ps.bitcast(BF16)
        oT = self.sb("oT", [128, 8, SEQ], BF16)
        oT_t = [[Trk() for _ in range(4)] for _ in range(8)]
        wq_v = self.na_w_qkv_d.rearrange("(kc p) f -> p kc f", p=128)
        with self.phase():
            wq = self.sb("wq", [128, 8, 128], BF16)
            wk = self.sb("wk", [128, 8, 128], BF16)
            wv = self.sb("wv", [128, 8, 128], BF16)
            w_t = Trk()
            qT = self.sb("qT", [128, SEQ], BF16)
            kT = self.sb("kT", [128, 2304], BF16)
            vA = self.sb("vA", [128, 18, 2, 65], BF16)
            otok = self.sb("otok", [128, 16, 128], BF16)
            q_t, k_t, v_t, otok_t = Trk(), Trk(), Trk(), Trk()
            BT = self.sb("BT", [128, 5, 640])
            BT_t = Trk()
            sbt = [self.sb(f"sbt{i}", [128, 640]) for i in range(2)]
            sbt_t = [Trk(), Trk()]
            PT = [self.sb(f"PT{i}", [128, 896], BF16) for i in range(2)]
            PT_t = [Trk(), Trk()]
            rden = [self.sb(f"rden{i}", [128, 1]) for i in range(2)]
            rden_t = [Trk(), Trk()]
            P.op("dve", lambda e: e.memset(vA[:], 1.0), w=[v_t])
            for hp in range(8):
                P.dma("pool", wq[:], wq_v[:, :, hp * 128:(hp + 1) * 128], w=[w_t], key="naw")
                P.dma("pool", wk[:], wq_v[:, :, 1024 + hp * 128:1024 + (hp + 1) * 128], w=[w_t], key="naw")
                P.dma("pool", wv[:], wq_v[:, :, 2048 + hp * 128:2048 + (hp + 1) * 128], w=[w_t], key="naw")
                it = 0
                for g in range(5):
                    gw = 512 if g < 4 else 256
                    for which in range(2):
                        if which == 0 and g == 4:
                            continue
                        bank = 6 + it % 2
                        it += 1
                        wsrc = wq if which == 0 else wk
                        for kc in range(8):
                            P.op("pe", lambda e, kc=kc, bank=bank, wsrc=wsrc, g=g, gw=gw: e.matmul(
                                ps[:, bank, 0:gw], wsrc[:, kc, :], hT[:, kc, g * 512:g * 512 + gw],
                                start=(kc == 0), stop=(kc == 7)), r=[w_t, hT_t[g]], w=[ps_t[bank]])
                        if which == 0:
                            P.op("act", lambda e, bank=bank, g=g, gw=gw: e.activation(
                                out=qT[:, g * 512:g * 512 + gw], in_=ps[:, bank, 0:gw], func=AF.Copy, scale=0.125),
                                r=[ps_t[bank]], w=[q_t])
                        else:
                            P.op("act", lambda e, bank=bank, g=g, gw=gw: e.activation(
                                out=kT[:, g * 512:g * 512 + gw], in_=ps[:, bank, 0:gw], func=AF.Copy),
                                r=[ps_t[bank]], w=[k_t])
                for t in range(18):
                    bank = 6 + t % 2
                    for kc in range(8):
                        P.op("pe", lambda e, kc=kc, bank=bank, t=t: e.matmul(
                            ps[:, bank, 0:128], hT[:, kc, t * 128:(t + 1) * 128], wv[:, kc, :],
                            start=(kc == 0), stop=(kc == 7)), r=[w_t, hT_t[t // 4]], w=[ps_t[bank]])
                    P.op("dve", lambda e, bank=bank, t=t: e.tensor_copy(
                        out=vA[:, t, :, 0:64], in_=ps[:, bank, 0:128].rearrange("p (h d) -> p h d", h=2)),
                        r=[ps_t[bank]], w=[v_t])
                units = [(h, p) for h in range(2) for p in range(16)]

                def st_step(u):
                    h, p = units[u]
                    i = u % 2
                    if p == 0:
                        head = hp * 2 + h
                        P.dma("sp", BT[:], self.na_bt_d[head], w=[BT_t], key="nabt")
                    Bp = min(max(2 * p - 4, 0), 22)
                    hr = slice(h * 64, (h + 1) * 64)
                    bA = 2 * i
                    for j in range(5):
                        tok = (Bp + 2 * j) * 64
                        dst = ps[:, bA, j * 128:(j + 1) * 128] if j < 4 else ps[:, bA + 1, 0:128]
                        P.op("pe", lambda e, dst=dst, tok=tok: e.matmul(
                            dst, kT[hr, tok:tok + 128], qT[hr, p * 128:(p + 1) * 128], start=True, stop=True),
                            r=[q_t, k_t], w=[ps_t[bA if j < 4 else bA + 1]])
                    for c in range(2):
                        P.op("pe", lambda e, c=c: e.matmul(
                            ps[:, bA + 1, 128 + c * 128:256 + c * 128], kT[hr, 2048 + c * 128:2176 + c * 128],
                            qT[hr, p * 128:(p + 1) * 128], start=True, stop=True), r=[q_t, k_t], w=[ps_t[bA + 1]])
                    var = {0: 0, 1: 1, 14: 3, 15: 4}.get(p, 2)
                    flat = ps[:, bA:bA + 2, :].rearrange("p a n -> p (a n)")
                    P.op("dve", lambda e: e.tensor_tensor(out=sbt[i][:], in0=flat[:, 0:640], in1=BT[:, var, :], op=ALU.add),
                         r=[ps_t[bA], ps_t[bA + 1], BT_t], w=[sbt_t[i]])
                    P.op("act", lambda e: e.activation(out=PT[i][:, 0:640], in_=sbt[i][:], func=AF.Exp),
                         r=[sbt_t[i]], w=[PT_t[i]])
                    P.op("act", lambda e: e.activation(out=PT[i][:, 640:896], in_=flat[:, 640:896], func=AF.Exp),
                         r=[ps_t[bA + 1]], w=[PT_t[i]])

                def pv_step(u):
                    h, p = units[u]
                    i = u % 2
                    Bp = min(max(2 * p - 4, 0), 22)
                    bO = 4 + i
                    tiles = [Bp // 2 + j for j in range(5)] + [16, 17]
                    for ci, tl in enumerate(tiles):
                        P.op("pe", lambda e, ci=ci, tl=tl: e.matmul(
                            ps[:, bO, 0:65], PT[i][:, ci * 128:(ci + 1) * 128], vA[:, tl, h, :],
                            start=(ci == 0), stop=(ci == 6)), r=[PT_t[i], v_t], w=[ps_t[bO]])
                    P.op("dve", lambda e: e.reciprocal(out=rden[i][:], in_=ps[:, bO, 64:65]), r=[ps_t[bO]], w=[rden_t[i]])
                    P.op("dve", lambda e: e.tensor_scalar(out=otok[:, p, h * 64:(h + 1) * 64], in0=ps[:, bO, 0:64],
                                                          scalar1=rden[i][:, 0:1], scalar2=None, op0=ALU.mult),
                         r=[ps_t[bO], rden_t[i]], w=[otok_t])

                st_step(0)
                for u in range(len(units)):
                    if u + 1 < len(units):
                        st_step(u + 1)
                    pv_step(u)
                for g in range(4):
                    bank = 6 + g % 2
                    for tt in range(4):
                        t = g * 4 + tt
                        P.op("pe", lambda e, bank=bank, tt=tt, t=t: e.transpose(
                            psb[:, bank, tt * 128:(tt + 1) * 128], otok[:, t, :], self.identb[:]),
                            r=[otok_t, self.cst_t], w=[ps_t[bank]])
                    P.op("act", lambda e, bank=bank, g=g: e.activation(
                        out=oT[:, hp, g * 512:(g + 1) * 512], in_=psb[:, bank, 0:512], func=AF.Copy),
                        r=[ps_t[bank]], w=[oT_t[hp][g]])
        with self.phase():
            wo = self.sb("wo", [128, 8, D], BF16)
            wo_t = Trk()
            P.dma("pool", wo[:], self.na_w_out_d.rearrange("(kc p) f -> p kc f", p=128), w=[wo_t], key="naw")
            gbc = self.sb("gbc1", [128, D])
            gbc_t = Trk()
            tmp = self.sb("tmp1", [128, D])
            tmp_t = Trk()
            self.bcast_cols(gbc[:], lambda kc: self.modT[:, l, 16 + kc, b:b + 1], gbc_t)
            for t in range(16):
                b0 = 4 + 2 * (t % 2)
                for hh in range(2):
                    for hp in range(8):
                        P.op("pe", lambda e, hh=hh, hp=hp, t=t, b0=b0: e.matmul(
                            ps[:, b0 + hh, :], oT[:, hp, t * 128:(t + 1) * 128], wo[:, hp, hh * 512:(hh + 1) * 512],
                            start=(hp == 0), stop=(hp == 7)), r=[oT_t[hp][t // 4], wo_t], w=[ps_t[b0 + hh]])
                self.resid_add(t, b0, gbc[:], gbc_t, tmp, tmp_t)


    def mixer_gla(self, b, l):
        nc, P = self.nc, self.P
        xs, xs_t, hT, hT_t, ps, ps_t = self.xs, self.xs_t, self.hT, self.hT_t, self.ps, self.ps_t
        psb = ps.bitcast(BF16)
        modT = self.modT
        win_v = self.gla_w_in_d.rearrange("(kc p) f -> p kc f", p=128)
        wout_v = self.gla_w_out_d.rearrange("(kc p) f -> p kc f", p=128)
        gwid = [512, 512, 512, 512, 256]
        gbc = self.sb("gbcA", [128, 2, D])
        gbc_t = [Trk(), Trk()]
        tmp = self.sb("tmpA", [128, D])
        tmp_t = Trk()
        self.bcast_cols(gbc[:, 0, :], lambda kc: modT[:, l, 16 + kc, b:b + 1], gbc_t[0])
        self.bcast_cols(gbc[:, 1, :], lambda kc: modT[:, l, 16 + kc, 4:5], gbc_t[1])
        gc = self.sb("gconst", [128, 772])
        gc_t = Trk()
        P.dma("sp", gc[:], self.gconst_d, w=[gc_t], key="gc")
        McF, McB, MsF, MsB = gc[:, 0:128], gc[:, 128:256], gc[:, 256:384], gc[:, 384:512]
        mkF, mkB, Ind = gc[:, 512:640], gc[:, 640:768], gc[:, 768:770]

        with self.phase():
            uT = self.sb("uT", [128, 4, 2078], BF16)
            uTc = self.sb("uTc", [128, 4, 286], BF16)
            u_t = Trk()
            P.op("dve", lambda e: e.memset(uT[:], 0.0), w=[u_t])
            P.op("dve", lambda e: e.memset(uTc[:], 0.0), w=[u_t])
            cvp = self.sb("cvp", [128, 4, 34])
            cvp_t = Trk()
            P.dma("sp", cvp[:], self.convp_d, w=[cvp_t], key="gc")
            with self.phase():
                wa = [self.sb(f"wa{i}", [128, 8, 128], BF16) for i in range(2)]
                wg_ = [self.sb(f"wgt{i}", [128, 8, 128], BF16) for i in range(2)]
                wa_t = [Trk(), Trk()]
                sg = [self.sb(f"sg{i}", [128, 512]) for i in range(2)]
                sg_t = [Trk(), Trk()]
                it = 0
                for j in range(4):
                    i = j % 2
                    P.dma("pool", wa[i][:], win_v[:, :, OFF_GLU + j * 128:OFF_GLU + (j + 1) * 128], w=[wa_t[i]], key=f"cw{i}")
                    P.dma("pool", wg_[i][:], win_v[:, :, OFF_GLU + 512 + j * 128:OFF_GLU + 512 + (j + 1) * 128], w=[wa_t[i]], key=f"cw{i}")
                    for g in range(5):
                        gw = gwid[g]
                        k2 = it % 2
                        it += 1
                        ba, bg = 2 * k2, 2 * k2 + 1
                        for kc in range(8):
                            P.op("pe", lambda e, kc=kc, g=g, gw=gw, ba=ba, i=i: e.matmul(
                                ps[:, ba, 0:gw], wa[i][:, kc, :], hT[:, kc, g * 512:g * 512 + gw], start=(kc == 0), stop=(kc == 7)),
                                r=[wa_t[i], hT_t[g]], w=[ps_t[ba]])
                        for kc in range(8):
                            P.op("pe", lambda e, kc=kc, g=g, gw=gw, bg=bg, i=i: e.matmul(
                                ps[:, bg, 0:gw], wg_[i][:, kc, :], hT[:, kc, g * 512:g * 512 + gw], start=(kc == 0), stop=(kc == 7)),
                                r=[wa_t[i], hT_t[g]], w=[ps_t[bg]])
                        P.op("act", lambda e, k2=k2, bg=bg, gw=gw: e.activation(out=sg[k2][:, 0:gw], in_=ps[:, bg, 0:gw], func=AF.Sigmoid),
                             r=[ps_t[bg]], w=[sg_t[k2]])
                        dst = uT[:, j, 15 + g * 512:15 + g * 512 + gw] if g < 4 else uTc[:, j, 15:15 + gw]
                        P.op("dve", lambda e, dst=dst, ba=ba, k2=k2, gw=gw: e.tensor_tensor(
                            out=dst, in0=ps[:, ba, 0:gw], in1=sg[k2][:, 0:gw], op=ALU.mult),
                            r=[ps_t[ba], sg_t[k2]], w=[u_t])
            with self.phase():
                NDG = 8
                dg = [self.sb(f"dg{i}", [128, 128], BF16) for i in range(NDG)]
                dg_t = [Trk() for _ in range(NDG)]
                yf = self.sb("yf", [128, 4, 512])
                ysq = self.sb("ysq", [128, 4, 512])
                yf_t = [Trk() for _ in range(4)]
                st1 = self.sb("st1", [128, 512])
                st2 = self.sb("st2", [128, 512])
                st_t = Trk()
                t1 = [self.sb(f"t1_{i}", [128, 512]) for i in range(2)]
                t1_t = [Trk(), Trk()]
                ycT = self.sb("ycT", [128, 4, 512], BF16)
                ycT_t = Trk()
                woc = self.sb("woc", [128, 4, D], BF16)
                woc_t = Trk()
                P.dma("pool", woc[:], wout_v[:, 4:8, :], w=[woc_t], key="cw0")
                dit = 0
                for g in range(5):
                    gw = gwid[g]
                    for j in range(4):
                        src = uT[:, j, :] if g < 4 else uTc[:, j, :]
                        off = g * 512 if g < 4 else 0
                        for tap in range(31):
                            di = dit % NDG
                            dit += 1
                            P.op("dve", lambda e, di=di, j=j, tap=tap: e.tensor_scalar(
                                out=dg[di][:], in0=self.identf[:], scalar1=cvp[:, j, tap:tap + 1], scalar2=None, op0=ALU.mult),
                                r=[self.cst_t, cvp_t], w=[dg_t[di]])
                            P.op("pe", lambda e, di=di, j=j, tap=tap, src=src, off=off, gw=gw: e.matmul(
                                ps[:, j, 0:gw], dg[di][:], src[:, off + tap:off + tap + gw], start=(tap == 0), stop=(tap == 30)),
                                r=[dg_t[di], u_t], w=[ps_t[j]])
                        P.op("act", lambda e, j=j, gw=gw: e.activation(out=yf[:, j, 0:gw], in_=ps[:, j, 0:gw], func=AF.Identity,
                                                                        bias=cvp[:, j, 31:32], scale=1.0),
                             r=[ps_t[j], cvp_t], w=[yf_t[j]])
                        P.op("act", lambda e, j=j, gw=gw: e.activation(out=ysq[:, j, 0:gw], in_=ps[:, j, 0:gw], func=AF.Square,
                                                                        bias=cvp[:, j, 31:32], scale=1.0),
                             r=[ps_t[j], cvp_t], w=[yf_t[j]])
                    for j in range(4):
                        P.op("pe", lambda e, j=j, gw=gw: e.matmul(ps[:, 4, 0:gw], self.onesm[:], yf[:, j, 0:gw], start=(j == 0), stop=(j == 3)),
                             r=[yf_t[j], self.cst_t], w=[ps_t[4]])
                    for j in range(4):
                        P.op("pe", lambda e, j=j, gw=gw: e.matmul(ps[:, 5, 0:gw], self.onesm[:], ysq[:, j, 0:gw], start=(j == 0), stop=(j == 3)),
                             r=[yf_t[j], self.cst_t], w=[ps_t[5]])
                    P.op("dve", lambda e, gw=gw: e.tensor_tensor(out=st1[:, 0:gw], in0=ps[:, 4, 0:gw], in1=ps[:, 4, 0:gw], op=ALU.mult)
                         if False else e.tensor_copy(out=st1[:, 0:gw], in_=ps[:, 4, 0:gw]), r=[ps_t[4]], w=[st_t])
                    P.op("dve", lambda e, gw=gw: e.tensor_tensor(out=st2[:, 0:gw], in0=st1[:, 0:gw], in1=st1[:, 0:gw], op=ALU.mult),
                         r=[st_t], w=[st_t])
                    P.op("dve", lambda e, gw=gw: e.tensor_tensor(out=st2[:, 0:gw], in0=ps[:, 5, 0:gw], in1=st2[:, 0:gw], op=ALU.subtract),
                         r=[ps_t[5], st_t], w=[st_t])
                    P.op("act", lambda e, gw=gw: e.activation(out=st2[:, 0:gw], in_=st2[:, 0:gw], func=AF.Sqrt, bias=self.epsc[:, 0:1], scale=1.0),
                         r=[st_t, self.cst_t], w=[st_t])
                    P.op("dve", lambda e, gw=gw: e.reciprocal(out=st2[:, 0:gw], in_=st2[:, 0:gw]), r=[st_t], w=[st_t])
                    for j in range(4):
                        i = j % 2
                        P.op("dve", lambda e, j=j, i=i, gw=gw: e.tensor_tensor(out=t1[i][:, 0:gw], in0=yf[:, j, 0:gw], in1=st1[:, 0:gw], op=ALU.subtract),
                             r=[yf_t[j], st_t], w=[t1_t[i]])
                        P.op("dve", lambda e, i=i, gw=gw: e.tensor_tensor(out=t1[i][:, 0:gw], in0=t1[i][:, 0:gw], in1=st2[:, 0:gw], op=ALU.mult),
                             r=[t1_t[i], st_t], w=[t1_t[i]])
                        P.op("act", lambda e, j=j, i=i, gw=gw: e.activation(out=ycT[:, j, 0:gw], in_=t1[i][:, 0:gw], func=AF.Silu,
                                                                             scale=cvp[:, j, 32:33], bias=cvp[:, j, 33:34]),
                             r=[t1_t[i], cvp_t], w=[ycT_t])
                    for tt in range(gw // 128):
                        t = g * 4 + tt
                        for hh in range(2):
                            for j in range(4):
                                P.op("pe", lambda e, hh=hh, j=j, tt=tt: e.matmul(
                                    ps[:, 6 + hh, :], ycT[:, j, tt * 128:(tt + 1) * 128], woc[:, j, hh * 512:(hh + 1) * 512],
                                    start=(j == 0), stop=(j == 3)), r=[ycT_t, woc_t], w=[ps_t[6 + hh]])
                        cx = 1 if t >= 16 else 0
                        self.resid_add(t, 6, gbc[:, cx, :], gbc_t[cx], tmp, tmp_t)

        if "convonly" in self.debug:
            return
        a_aug = self.sb("a_aug", [33, 2304], BF16)
        a_t = Trk()
        wabd = self.sb("wabd", [33, 512], BF16)
        wabd_t = Trk()
        P.dma("pool", wabd[:], self.wabd_d, w=[wabd_t], key="cw0")
        ngb = self.sb("ngb", [128, 512])
        ngb_t = Trk()
        P.dma("sp", ngb[:], self.gla_norm_g_d.partition_broadcast(128), w=[ngb_t], key="gc")
        cln = self.sb("cln", [128, 2])
        P.op("dve", lambda e: e.memset(cln[:, 0:1], float(np.log(0.125))), w=[a_t])
        P.op("dve", lambda e: e.memset(cln[:, 1:2], 1.0), w=[a_t])
        P.op("dve", lambda e: e.memset(a_aug[32:33, :], 1.0), w=[a_t])
        with self.phase():
            waf = self.sb("waf", [128, 8, 32], BF16)
            waf_t = Trk()
            P.dma("pool", waf[:], win_v[:, :, OFF_AF:OFF_AF + 32], w=[waf_t], key="cw1")
            for g in range(5):
                gw = gwid[g]
                for kc in range(8):
                    P.op("pe", lambda e, kc=kc, g=g, gw=gw: e.matmul(ps[0:32, g % 2, 0:gw], waf[:, kc, :], hT[:, kc, g * 512:g * 512 + gw],
                                                                  start=(kc == 0), stop=(kc == 7)), r=[waf_t, hT_t[g]], w=[ps_t[g % 2]])
                P.op("act", lambda e, g=g, gw=gw: e.activation(out=a_aug[0:32, g * 512:g * 512 + gw], in_=ps[0:32, g % 2, 0:gw], func=AF.Copy),
                     r=[ps_t[g % 2]], w=[a_t])
        for hp in range(2):
            with self.phase():
                qr = self.sb("qr", [128, 18, 128], BF16)
                kr = self.sb("kr", [128, 18, 128], BF16)
                vv = self.sb("vv", [128, 18, 256], BF16)
                qkv_t = [Trk() for _ in range(18)]
                oacc = self.sb("oacc", [128, 18, 256])
                oacc_t = [Trk() for _ in range(18)]
                with self.phase():
                    wq = self.sb("wq0", [128, 8, 128], BF16)
                    wk = self.sb("wk0", [128, 8, 128], BF16)
                    wv = self.sb("wv0", [128, 8, 256], BF16)
                    w_t = Trk()
                    P.dma("pool", wq[:], win_v[:, :, OFF_Q + hp * 128:OFF_Q + (hp + 1) * 128], w=[w_t], key="cw0")
                    P.dma("pool", wk[:], win_v[:, :, OFF_K + hp * 128:OFF_K + (hp + 1) * 128], w=[w_t], key="cw0")
                    P.dma("pool", wv[:], win_v[:, :, OFF_V + hp * 256:OFF_V + (hp + 1) * 256], w=[w_t], key="cw0")
                    rC = self.sb("rC", [128, 18, 64])
                    rS = self.sb("rS", [128, 18, 64])
                    rope_t = Trk()
                    P.dma("sp", rC[:], self.ropeC_d, w=[rope_t], key="gc")
                    P.dma("act", rS[:], self.ropeS_d, w=[rope_t], key="gc2")
                    rt = [self.sb(f"rt{i}", [128, 128]) for i in range(2)]
                    rt2 = [self.sb(f"rt2{i}", [128, 128]) for i in range(2)]
                    rt_t = [Trk(), Trk()]
                    it = 0
                    for t in range(18):
                        for which in range(2):
                            i = it % 2
                            it += 1
                            bank = i
                            wsrc = wq if which == 0 else wk
                            for kc in range(8):
                                P.op("pe", lambda e, kc=kc, bank=bank, wsrc=wsrc, t=t: e.matmul(
                                    ps[:, bank, 0:128], hT[:, kc, t * 128:(t + 1) * 128], wsrc[:, kc, :], start=(kc == 0), stop=(kc == 7)),
                                    r=[w_t, hT_t[t // 4]], w=[ps_t[bank]])
                            x5 = ps[:, bank, 0:128].rearrange("p (h a w i) -> p h a w i", h=2, a=2, w=2, i=16)
                            S4 = rS[:, t, :].rearrange("p (a w i) -> p a w i", a=2, w=2, i=16)
                            tm5 = rt[i][:].rearrange("p (h a w i) -> p h a w i", h=2, a=2, w=2, i=16)
                            for w_ in range(2):
                                P.op("dve", lambda e, x5=x5, S4=S4, tm5=tm5, w_=w_: e.tensor_tensor(
                                    out=tm5[:, :, :, w_, :], in0=x5[:, :, :, 1 - w_, :],
                                    in1=S4[:, :, w_, :].unsqueeze(1).to_broadcast([128, 2, 2, 16]), op=ALU.mult),
                                    r=[ps_t[bank], rope_t], w=[rt_t[i]])
                            P.op("dve", lambda e, bank=bank, i=i, t=t: e.tensor_tensor(
                                out=rt2[i][:].rearrange("p (h d) -> p h d", h=2), in0=ps[:, bank, 0:128].rearrange("p (h d) -> p h d", h=2),
                                in1=rC[:, t, :].unsqueeze(1).to_broadcast([128, 2, 64]), op=ALU.mult),
                                r=[ps_t[bank], rope_t], w=[rt_t[i]])
                            dst = qr if which == 0 else kr
                            P.op("dve", lambda e, i=i, dst=dst, t=t: e.tensor_tensor(out=dst[:, t, :], in0=rt2[i][:], in1=rt[i][:], op=ALU.add),
                                 r=[rt_t[i]], w=[qkv_t[t]])
                        bank = 2 + t % 2
                        for kc in range(8):
                            P.op("pe", lambda e, kc=kc, bank=bank, t=t: e.matmul(
                                ps[:, bank, 0:256], hT[:, kc, t * 128:(t + 1) * 128], wv[:, kc, :], start=(kc == 0), stop=(kc == 7)),
                                r=[w_t, hT_t[t // 4]], w=[ps_t[bank]])
                        P.op("act", lambda e, bank=bank, t=t: e.activation(out=vv[:, t, :], in_=ps[:, bank, 0:256], func=AF.Copy),
                             r=[ps_t[bank]], w=[qkv_t[t]])
                with self.phase():
                    wgg = self.sb("wgg", [128, 8, 256], BF16)
                    wog = self.sb("wog", [128, 2, D], BF16)
                    wg_t = Trk()
                    P.dma("pool", wgg[:], win_v[:, :, OFF_G + hp * 256:OFF_G + (hp + 1) * 256], w=[wg_t], key="cw0")
                    P.dma("pool", wog[:], wout_v[:, hp * 2:hp * 2 + 2, :], w=[wg_t], key="cw0")
                    S = self.sb("S", [128, 128])
                    Sb = self.sb("Sb", [128, 128], BF16)
                    S_t = Trk()
                    nb = 2
                    e1 = [self.sb(f"e1_{i}", [128, 128]) for i in range(nb)]
                    la = [self.sb(f"la_{i}", [128, 128]) for i in range(nb)]
                    Eq = [self.sb(f"Eq_{i}", [128, 128]) for i in range(nb)]
                    Ek = [self.sb(f"Ek_{i}", [128, 128]) for i in range(nb)]
                    Ed = [self.sb(f"Ed_{i}", [128, 128]) for i in range(nb)]
                    ebl = [self.sb(f"ebl_{i}", [128, 2]) for i in range(nb)]
                    qt = [self.sb(f"qt_{i}", [128, 128], BF16) for i in range(nb)]
                    kt = [self.sb(f"kt_{i}", [128, 128], BF16) for i in range(nb)]
                    kd = [self.sb(f"kd_{i}", [128, 128], BF16) for i in range(nb)]
                    qtT = [self.sb(f"qtT_{i}", [128, 128], BF16) for i in range(nb)]
                    ktT = [self.sb(f"ktT_{i}", [128, 128], BF16) for i in range(nb)]
                    attm = [[self.sb(f"attm_{i}_{h}", [128, 128], BF16) for h in range(2)] for i in range(nb)]
                    tl_t = [Trk() for _ in range(nb)]
                    ex_t = [Trk() for _ in range(nb)]
                    qk_t = [Trk() for _ in range(nb)]
                    tr_t = [Trk() for _ in range(nb)]
                    at_t = [[Trk(), Trk()] for _ in range(nb)]
                    ssq = self.sb("ssq", [128, 4])
                    junk = self.sb("junk", [128, 128])
                    sgt = self.sb("sgt", [128, 256])
                    ot = self.sb("ot", [128, 256])
                    yg = self.sb("yg", [128, 256], BF16)
                    ygT = self.sb("ygT", [128, 2, 128], BF16)
                    po_t = Trk()
                    tix = 0
                    for d in range(2):
                        Mc, Ms, mk = (McF, MsF, mkF) if d == 0 else (McB, MsB, mkB)
                        zc0 = d * 256 + hp * 128
                        seqs = ([16, 17], list(range(16))) if d == 0 else ([17, 16], list(range(15, -1, -1)))
                        for si, seq in enumerate(seqs):
                            if si == 0:
                                P.op("dve", lambda e: e.memset(S[:], 0.0), w=[S_t])
                                P.op("dve", lambda e: e.memset(Sb[:], 0.0), w=[S_t])
                            for t in seq:
                                i = tix % nb
                                tix += 1
                                P.op("pe", lambda e, t=t: e.matmul(ps[:, 0, 0:128], a_aug[:, t * 128:(t + 1) * 128], wabd[:, zc0:zc0 + 128],
                                                                  start=True, stop=True), r=[a_t, wabd_t], w=[ps_t[0]])
                                P.op("act", lambda e, i=i: e.activation(out=e1[i][:], in_=ps[:, 0, 0:128], func=AF.Exp, scale=-1.0),
                                     r=[ps_t[0]], w=[tl_t[i]])
                                P.op("act", lambda e, i=i: e.activation(out=la[i][:], in_=e1[i][:], func=AF.Ln, bias=cln[:, 1:2], scale=1.0),
                                     r=[tl_t[i], a_t], w=[tl_t[i]])
                                P.op("pe", lambda e, i=i: e.matmul(ps[:, 0, 128:256], Mc, la[i][:], start=True, stop=True),
                                     r=[tl_t[i], gc_t], w=[ps_t[0]])
                                P.op("pe", lambda e, i=i: e.matmul(ps[:, 1, 0:128], Ms, la[i][:], start=True, stop=True),
                                     r=[tl_t[i], gc_t], w=[ps_t[1]])
                                P.op("pe", lambda e, i=i: e.matmul(ps[:, 2, 0:2], la[i][:], Ind, start=True, stop=True),
                                     r=[tl_t[i], gc_t], w=[ps_t[2]])
                                P.op("act", lambda e, i=i: e.activation(out=Eq[i][:], in_=ps[:, 0, 128:256], func=AF.Exp, bias=cln[:, 0:1], scale=1.0),
                                     r=[ps_t[0], a_t], w=[ex_t[i]])
                                P.op("act", lambda e, i=i: e.activation(out=Ek[i][:], in_=ps[:, 0, 128:256], func=AF.Exp, scale=-1.0),
                                     r=[ps_t[0]], w=[ex_t[i]])
                                P.op("act", lambda e, i=i: e.activation(out=Ed[i][:], in_=ps[:, 1, 0:128], func=AF.Exp),
                                     r=[ps_t[1]], w=[ex_t[i]])
                                P.op("act", lambda e, i=i: e.activation(out=ebl[i][:], in_=ps[:, 2, 0:2], func=AF.Exp),
                                     r=[ps_t[2]], w=[ex_t[i]])
                                P.op("dve", lambda e, i=i, t=t: e.tensor_tensor(out=qt[i][:], in0=qr[:, t, :], in1=Eq[i][:], op=ALU.mult),
                                     r=[qkv_t[t], ex_t[i]], w=[qk_t[i]])
                                P.op("dve", lambda e, i=i, t=t: e.tensor_tensor(out=kt[i][:], in0=kr[:, t, :], in1=Ek[i][:], op=ALU.mult),
                                     r=[qkv_t[t], ex_t[i]], w=[qk_t[i]])
                                P.op("dve", lambda e, i=i, t=t: e.tensor_tensor(out=kd[i][:], in0=kr[:, t, :], in1=Ed[i][:], op=ALU.mult),
                                     r=[qkv_t[t], ex_t[i]], w=[qk_t[i]])
                                P.op("pe", lambda e, i=i: e.transpose(psb[:, 2, 128:256], qt[i][:], self.identb[:]),
                                     r=[qk_t[i], self.cst_t], w=[ps_t[2]])
                                P.op("pe", lambda e, i=i: e.transpose(psb[:, 2, 256:384], kt[i][:], self.identb[:]),
                                     r=[qk_t[i], self.cst_t], w=[ps_t[2]])
                                P.op("act", lambda e, i=i: e.activation(out=qtT[i][:], in_=psb[:, 2, 128:256], func=AF.Copy),
                                     r=[ps_t[2]], w=[tr_t[i]])
                                P.op("act", lambda e, i=i: e.activation(out=ktT[i][:], in_=psb[:, 2, 256:384], func=AF.Copy),
                                     r=[ps_t[2]], w=[tr_t[i]])
                                for h in range(2):
                                    hr = slice(h * 64, (h + 1) * 64)
                                    P.op("pe", lambda e, i=i, h=h, hr=hr: e.matmul(ps[:, 3 + h, 0:128], ktT[i][hr, :], qtT[i][hr, :], start=True, stop=True),
                                         r=[tr_t[i]], w=[ps_t[3 + h]])
                                    P.op("dve", lambda e, i=i, h=h: e.tensor_tensor(out=attm[i][h][:], in0=ps[:, 3 + h, 0:128], in1=mk, op=ALU.mult),
                                         r=[ps_t[3 + h], gc_t], w=[at_t[i][h]])
                                for c in ((0, 1) if d == 0 else (1, 0)):
                                    cr = slice(c * 64, (c + 1) * 64)
                                    for h in range(2):
                                        hr = slice(h * 64, (h + 1) * 64)
                                        P.op("pe", lambda e, i=i, h=h, cr=cr, t=t: e.matmul(
                                            ps[cr, 5, h * 128:(h + 1) * 128], attm[i][h][:, cr], vv[:, t, h * 128:(h + 1) * 128], start=True, stop=False),
                                            r=[at_t[i][h], qkv_t[t]], w=[ps_t[5]])
                                        P.op("pe", lambda e, i=i, h=h, cr=cr, hr=hr: e.matmul(
                                            ps[cr, 5, h * 128:(h + 1) * 128], qtT[i][hr, cr], Sb[hr, :], start=False, stop=True),
                                            r=[tr_t[i], S_t], w=[ps_t[5]])
                                    P.op("pe", lambda e, i=i, cr=cr, t=t: e.matmul(ps[:, 6, 0:256], kd[i][cr, :], vv[cr, t, :], start=True, stop=True),
                                         r=[qk_t[i], qkv_t[t]], w=[ps_t[6]])
                                    for h in range(2):
                                        hr = slice(h * 64, (h + 1) * 64)
                                        P.op("dve", lambda e, i=i, h=h, hr=hr, c=c: e.scalar_tensor_tensor(
                                            out=S[hr, :], in0=S[hr, :], scalar=ebl[i][hr, c:c + 1], in1=ps[hr, 6, h * 128:(h + 1) * 128],
                                            op0=ALU.mult, op1=ALU.add), r=[S_t, ex_t[i], ps_t[6]], w=[S_t])
                                    P.op("act", lambda e: e.activation(out=Sb[:], in_=S[:], func=AF.Copy), r=[S_t], w=[S_t])
                                if d == 0:
                                    P.op("act", lambda e, t=t: e.activation(out=oacc[:, t, :], in_=ps[:, 5, 0:256], func=AF.Copy),
                                         r=[ps_t[5]], w=[oacc_t[t]])
                                    continue
                                P.op("dve", lambda e, t=t: e.tensor_tensor(out=ot[:], in0=ps[:, 5, 0:256], in1=oacc[:, t, :], op=ALU.add),
                                     r=[ps_t[5], oacc_t[t]], w=[po_t])
                                for h in range(2):
                                    P.op("act", lambda e, h=h: e.activation(out=junk[:], in_=ot[:, h * 128:(h + 1) * 128], func=AF.Square,
                                                                           accum_out=ssq[:, h:h + 1]), r=[po_t], w=[po_t])
                                P.op("act", lambda e: e.activation(out=ssq[:, 2:4], in_=ssq[:, 0:2], func=AF.Sqrt, scale=1.0 / 128, bias=self.epsc[:, 0:1]),
                                     r=[po_t, self.cst_t], w=[po_t])
                                P.op("dve", lambda e: e.reciprocal(out=ssq[:, 2:4], in_=ssq[:, 2:4]), r=[po_t], w=[po_t])
                                for kc in range(8):
                                    P.op("pe", lambda e, kc=kc, t=t: e.matmul(ps[:, 7, 0:256], hT[:, kc, t * 128:(t + 1) * 128], wgg[:, kc, :],
                                                                           start=(kc == 0), stop=(kc == 7)), r=[wg_t, hT_t[t // 4]], w=[ps_t[7]])
                                P.op("act", lambda e: e.activation(out=sgt[:], in_=ps[:, 7, 0:256], func=AF.Silu), r=[ps_t[7]], w=[po_t])
                                for h in range(2):
                                    P.op("dve", lambda e, h=h: e.tensor_scalar(out=ot[:, h * 128:(h + 1) * 128], in0=ot[:, h * 128:(h + 1) * 128],
                                                                               scalar1=ssq[:, 2 + h:3 + h], scalar2=None, op0=ALU.mult),
                                         r=[po_t], w=[po_t])
                                P.op("dve", lambda e: e.tensor_tensor(out=ot[:], in0=ot[:], in1=ngb[:, hp * 256:(hp + 1) * 256], op=ALU.mult),
                                     r=[po_t, ngb_t], w=[po_t])
                                P.op("dve", lambda e: e.tensor_tensor(out=yg[:], in0=ot[:], in1=sgt[:], op=ALU.mult), r=[po_t], w=[po_t])
                                for h in range(2):
                                    P.op("pe", lambda e, h=h: e.transpose(psb[:, 2, 512 + h * 128:640 + h * 128], yg[:, h * 128:(h + 1) * 128], self.identb[:]),
                                         r=[po_t, self.cst_t], w=[ps_t[2]])
                                P.op("act", lambda e: e.activation(out=ygT[:].rearrange("p h t -> p (h t)"), in_=psb[:, 2, 512:768], func=AF.Copy),
                                     r=[ps_t[2]], w=[po_t])
                                for hh in range(2):
                                    for h in range(2):
                                        P.op("pe", lambda e, hh=hh, h=h: e.matmul(ps[:, 6 + hh, :], ygT[:, h, :], wog[:, h, hh * 512:(hh + 1) * 512],
                                                                               start=(h == 0), stop=(h == 1)), r=[po_t, wg_t], w=[ps_t[6 + hh]])
                                cx = 1 if t >= 16 else 0
                                self.resid_add(t, 6, gbc[:, cx, :], gbc_t[cx], tmp, tmp_t)

    def sample(self, b):
        nc, P = self.nc, self.P
        xs, xs_t = self.xs, self.xs_t
        for t in range(16):
            q = "sp" if t % 2 == 0 else "act"
            P.dma(q, xs[:, t, :], self.x_d[b, t * 128:(t + 1) * 128, :], w=[xs_t[t]], key=f"x{t}")
        for t in range(2):
            P.dma("sp", xs[:, 16 + t, :], self.ctx_d[b, t * 128:(t + 1) * 128, :], w=[xs_t[16 + t]], key=f"x{16 + t}")
        if "glatest" in self.debug:
            with self.phase():
                self.prenorm(b, 0, 0, list(range(18)))
            with self.phase():
                self.mixer_gla(b, 0)
            if b == 0:
                for t in range(18):
                    P.dma("sp", self.dbg_xs[:, t, :], xs[:, t, :], r=[xs_t[t]], w=[Trk()], key="dbg")
            return
        if "natest" in self.debug:
            with self.phase():
                self.prenorm(b, 1, 0, list(range(18)))
            with self.phase():
                self.mixer_na(b, 1)
            if b == 0:
                for t in range(18):
                    P.dma("sp", self.dbg_xs[:, t, :], xs[:, t, :], r=[xs_t[t]], w=[Trk()], key="dbg")
            return
        if "moetest" in self.debug:
            with self.phase():
                self.prenorm(b, 0, 1, list(range(18)))
            with self.phase():
                self.moe(b, 0, 18)
            if b == 0:
                for t in range(18):
                    P.dma("sp", self.dbg_xs[:, t, :], xs[:, t, :], r=[xs_t[t]], w=[Trk()], key="dbg")
            return
        with self.phase():
            self.prenorm(b, 0, 0, list(range(18)))
        with self.phase():
            self.mixer_gla(b, 0)
        with self.phase():
            self.prenorm(b, 0, 1, list(range(18)))
        with self.phase():
            self.moe(b, 0, 18)
        with self.phase():
            self.prenorm(b, 1, 0, list(range(18)))
        with self.phase():
            self.mixer_na(b, 1)
        with self.phase():
            self.prenorm(b, 1, 1, list(range(16)))
        with self.phase():
            self.moe(b, 1, 16)
        with self.phase():
            fng = self.sb("fng_sb", [128, D])
            fng_t = Trk()
            P.dma("sp", fng[:], self.fng_d.partition_broadcast(128), w=[fng_t], key="gc")
            junk = self.sb("fjunk", [128, D], BF16)
            junk_t = Trk()
            fss = [self.sb(f"fss{i}", [128, 2]) for i in range(2)]
            fss_t = [Trk(), Trk()]
            for t in range(16):
                i = t % 2
                P.op("act", lambda e, t=t, i=i: e.activation(out=junk[:], in_=xs[:, t, :], func=AF.Square, accum_out=fss[i][:, 0:1]),
                     r=[xs_t[t]], w=[junk_t, fss_t[i]])
                P.op("act", lambda e, i=i: e.activation(out=fss[i][:, 1:2], in_=fss[i][:, 0:1], func=AF.Sqrt, scale=1.0 / D,
                                                        bias=self.epsc[:, 0:1]), r=[fss_t[i], self.cst_t], w=[fss_t[i]])
                P.op("dve", lambda e, i=i: e.reciprocal(out=fss[i][:, 1:2], in_=fss[i][:, 1:2]), r=[fss_t[i]], w=[fss_t[i]])
                P.op("dve", lambda e, t=t, i=i: e.scalar_tensor_tensor(out=xs[:, t, :], in0=xs[:, t, :], scalar=fss[i][:, 1:2],
                                                                       in1=fng[:], op0=ALU.mult, op1=ALU.mult),
                     r=[xs_t[t], fss_t[i], fng_t], w=[xs_t[t]])
                ot = Trk()
                P.dma("sp" if t % 2 == 0 else "act", self.out_d[b, t * 128:(t + 1) * 128, :], xs[:, t, :], r=[xs_t[t]], w=[ot],
                      key=f"o{t}")
                self.out_trks.append(ot)
        return
        for t in range(16):
            ot = Trk()
            P.dma("sp", self.out_d[b, t * 128:(t + 1) * 128, :], xs[:, t, :], r=[xs_t[t]], w=[ot], key=f"o{t % 4}")
            self.out_trks.append(ot)


_BT_CACHE = {}


def gla_consts():
    s_ = np.arange(128)[:, None]
    t_ = np.arange(128)[None, :]
    same = (s_ // 64) == (t_ // 64)
    c = np.zeros((128, 772), np.float32)
    c[:, 0:128] = np.where(same & (s_ <= t_), -1.0 / 16, 0.0)
    c[:, 128:256] = np.where(same & (s_ >= t_), -1.0 / 16, 0.0)
    c[:, 256:384] = np.where(same & (s_ > t_), -1.0 / 16, 0.0)
    c[:, 384:512] = np.where(same & (s_ < t_), -1.0 / 16, 0.0)
    c[:, 512:640] = np.where(same & (s_ <= t_), 1.0, 0.0)
    c[:, 640:768] = np.where(same & (s_ >= t_), 1.0, 0.0)
    c[0:64, 768] = -1.0 / 16
    c[64:128, 769] = -1.0 / 16
    return c


def rope_tables():
    tok = np.arange(SEQ)
    row = (tok // 64).astype(np.float32)
    col = (tok % 64).astype(np.float32)
    half = 16
    inv = (10000.0 ** (-np.arange(half, dtype=np.float32) / half)).astype(np.float32)
    C = np.ones((2304, 64), np.float32)
    S = np.zeros((2304, 64), np.float32)
    for a, pos in enumerate((row, col)):
        ang = pos[:, None] * inv[None, :]
        cs, sn = np.cos(ang).astype(np.float32), np.sin(ang).astype(np.float32)
        C[:SEQ, a * 32:a * 32 + 16] = cs
        C[:SEQ, a * 32 + 16:a * 32 + 32] = cs
        S[:SEQ, a * 32:a * 32 + 16] = -sn
        S[:SEQ, a * 32 + 16:a * 32 + 32] = sn
    f = lambda z: np.ascontiguousarray(z.reshape(18, 128, 64).transpose(1, 0, 2))
    return f(C), f(S)


def na_bias_table(rpb):
    key = rpb.tobytes()
    if key in _BT_CACHE:
        return _BT_CACHE[key]
    rpb = np.asarray(rpb, dtype=np.float32)
    kk = np.arange(128)
    krl, kc = kk // 64, kk % 64
    qq = np.arange(128)
    qrl, qc = qq // 64, qq % 64
    out = np.empty((16, 128, 5, 5, 128), np.float32)
    for vi, p in enumerate((0, 1, 2, 14, 15)):
        Bp = min(max(2 * p - 4, 0), 22)
        qrow = 2 * p + qrl
        rs = np.clip(qrow - 4, 0, 24)
        cs = np.clip(qc - 8, 0, 48)
        for j in range(5):
            krow = Bp + 2 * j + krl
            valid = ((krow[:, None] >= rs[None, :]) & (krow[:, None] < rs[None, :] + 8)
                     & (kc[:, None] >= cs[None, :]) & (kc[:, None] < cs[None, :] + 16))
            dr = np.clip(krow[:, None] - qrow[None, :] + 7, 0, 14)
            dc = np.clip(kc[:, None] - qc[None, :] + 15, 0, 30)
            vals = rpb[:, dr, dc]
            out[:, :, vi, j, :] = np.where(valid[None], vals, np.float32(-30000.0))
    res = np.ascontiguousarray(out.reshape(16, 128, 5, 640))
    _BT_CACHE[key] = res
    return res


def host_layouts(inp, core):
    b0 = core * NS
    f = lambda a: np.ascontiguousarray(a, dtype=np.float32)
    m = {}
    m["x"] = f(inp["x"][b0:b0 + NS])
    m["ctx"] = f(inp["ctx"][b0:b0 + NS])
    c5 = np.concatenate([inp["c"][b0:b0 + NS], inp["c_ctx"][None, :]], axis=0)
    m["cT"] = f(c5.T.reshape(8, 128, 5).transpose(1, 0, 2))
    m["ada_w"] = f(inp["ada_w"])
    m["ada_bT"] = f(inp["ada_b"].reshape(2, 48, 128).transpose(0, 2, 1))
    m["n1gT"] = f(inp["norm1_g"].reshape(2, 8, 128).transpose(0, 2, 1))
    m["n2gT"] = f(inp["norm2_g"].reshape(2, 8, 128).transpose(0, 2, 1))
    m["fng"] = f(inp["final_norm_g"])
    m["ident"] = np.eye(128, dtype=np.float32)
    m["na_w_qkv"] = f(inp["na_w_qkv"][0])
    m["gla_w_in"] = f(inp["gla_conv_w_in"][0])
    m["gla_w_out"] = f(inp["gla_conv_w_out"][0])
    m["gconst"] = gla_consts()
    cvp = np.zeros((128, 4, 34), np.float32)
    cvp[:, :, 0:31] = inp["conv_dw_w"][0].reshape(31, 4, 128).transpose(2, 1, 0)
    cvp[:, :, 31] = inp["conv_dw_b"][0].reshape(4, 128).T
    cvp[:, :, 32] = inp["conv_ln_g"][0].reshape(4, 128).T
    cvp[:, :, 33] = inp["conv_ln_b"][0].reshape(4, 128).T
    m["convp"] = cvp
    wabd = np.zeros((33, 512), np.float32)
    wabd[0:16, 0:256] = inp["gla_wa_fwd"][0]
    wabd[16:32, 256:512] = inp["gla_wa_bwd"][0]
    wabd[32, 0:256] = inp["gla_ba_fwd"][0]
    wabd[32, 256:512] = inp["gla_ba_bwd"][0]
    m["wabd"] = wabd
    m["gla_norm_g"] = f(inp["gla_norm_g"][0])
    rc, rs_ = rope_tables()
    m["ropeC"] = rc
    m["ropeS"] = rs_
    m["na_w_out"] = f(inp["na_w_out"][0])
    m["na_bt"] = na_bias_table(inp["na_rpb"][0])
    m["router_w"] = f(inp["router_w"])
    m["router_b"] = f(inp["router_b"])
    m["bguT"] = f(inp["expert_b_gu"].reshape(2, NE, 16, 128).transpose(0, 3, 1, 2))
    m["expert_b_down"] = f(inp["expert_b_down"])
    m["expert_w_gu"] = f(inp["expert_w_gu"])
    m["expert_w_down"] = f(inp["expert_w_down"])
    return m


_CACHE = {}


def kernel(**inputs):
    inp = {k: np.asarray(v) for k, v in inputs.items()}
    if "nc" not in _CACHE:
        bld = Builder()
        _CACHE["nc"] = bld.build()
    nc = _CACHE["nc"]
    in_maps = [host_layouts(inp, c) for c in range(NCORES)]
    res = run_bass_kernel_spmd(nc, in_maps, core_ids=list(range(NCORES)))
    out = np.concatenate([r["out"] for r in res.results], axis=0)
    return out.astype(np.float32)
```

```python
import numpy as np
from contextlib import ExitStack
import concourse.bass as bass
import concourse.mybir as mybir
from concourse.bass_utils import run_bass_kernel_spmd

F32 = mybir.dt.float32
BF16 = mybir.dt.bfloat16
AF = mybir.ActivationFunctionType
ALU = mybir.AluOpType
AX = mybir.AxisListType

D = 1024
SEQ = 2048
CTX = 256
NCORES = 8
NS = 4
EPS = 1e-6
NE = 32
W_IN_A = 2592
OFF_Q, OFF_G, OFF_GLU, OFF_K, OFF_V, OFF_AF, OFF_AB = 0, 256, 768, 1792, 2048, 2560, 2576


class Trk:
    __slots__ = ("w", "r")

    def __init__(self):
        self.w = None
        self.r = {}


class Eng:
    def __init__(self, name, be, sem, is_dma_only=False):
        self.name = name
        self.be = be
        self.sem = sem
        self.count = 0
        self.waited = {}


class Prog:
    def __init__(self, nc, es):
        self.nc = nc
        self.es = es
        self.E = {}
        for name, be in (("pe", nc.tensor), ("act", nc.scalar), ("dve", nc.vector),
                         ("pool", nc.gpsimd), ("sp", nc.sync)):
            sem = es.enter_context(nc.semaphore("s_" + name))
            self.E[name] = Eng(name, be, sem)
        self.dsem = {}
        self.semobj = {}
        for e in self.E.values():
            self.semobj[id(e.sem)] = e.sem
        self.ninstr = 0

    def _waits(self, e, r, w, is_dma):
        deps = {}

        def add(ev):
            k, v, en = ev
            if deps.get(k, (0, None))[0] < v:
                deps[k] = (v, en)

        for t in r:
            if t.w is not None:
                add(t.w)
        for t in w:
            if t.w is not None:
                if is_dma or t.w[2] != e.name:
                    add(t.w)
            for k, (v, en) in t.r.items():
                if is_dma or en != e.name:
                    add((k, v, en))
        out = []
        for k, (v, en) in deps.items():
            if e.waited.get(k, 0) >= v:
                continue
            e.waited[k] = v
            out.append((self.semobj[k], v))
        return out

    def _mark(self, ev, r, w):
        for t in w:
            t.w = ev
            t.r = {}
        for t in r:
            k, v, en = ev
            if t.r.get(k, (0, None))[0] < v:
                t.r[k] = (v, en)

    def _emit(self, en, waits, fn, sem, inc):
        e = self.E[en]

        def go():
            for so, v in waits:
                e.be.wait_ge(so, v)
            ins = fn(e.be)
            ins.then_inc(sem, inc)

        if getattr(self, "_cond", None) is not None:
            assert en in self._cond["engs"], en
            self._cond["rec"][en].append(go)
        else:
            go()

    def op(self, en, fn, r=(), w=()):
        e = self.E[en]
        waits = self._waits(e, r, w, False)
        e.count += 1
        self._emit(en, waits, fn, e.sem, 1)
        ev = (id(e.sem), e.count, en)
        self._mark(ev, r, w)
        self.ninstr += 1
        return ev

    def _dkey(self, key):
        if key not in self.dsem:
            sem = self.es.enter_context(self.nc.semaphore("d_" + key))
            self.dsem[key] = [sem, 0]
            self.semobj[id(sem)] = sem
        return self.dsem[key]

    def dma(self, qn, out, in_, r=(), w=(), key=None, **kw):
        e = self.E[qn]
        if key is None:
            key = "dflt_" + qn
        ent = self._dkey(key)
        waits = self._waits(e, r, w, True)
        if getattr(self, "_cond", None) is not None:
            self._cond["dma_eng"][key] = qn
        ent[1] += 16
        self._emit(qn, waits, lambda be: be.dma_start(out=out, in_=in_, **kw), ent[0], 16)
        ev = (id(ent[0]), ent[1], "dma_" + key)
        self._mark(ev, r, w)
        self.ninstr += 1
        return ev

    def idma(self, qn, out, out_off, in_, in_off, r=(), w=(), key=None):
        e = self.E[qn]
        ent = self._dkey(key)
        waits = self._waits(e, r, w, True)
        if getattr(self, "_cond", None) is not None:
            self._cond["dma_eng"][key] = qn
        ent[1] += 16
        self._emit(qn, waits, lambda be: be.indirect_dma_start(out=out, out_offset=out_off, in_=in_, in_offset=in_off), ent[0], 16)
        ev = (id(ent[0]), ent[1], "dma_" + key)
        self._mark(ev, r, w)
        self.ninstr += 1
        return ev

    def cond_begin(self, regs, thresh, engs=("pe", "act", "dve", "sp")):
        self._cond = dict(counts={en: self.E[en].count for en in engs}, dcounts={k: v[1] for k, v in self.dsem.items()},
                          waited={en: dict(self.E[en].waited) for en in engs}, dma_eng={}, engs=engs,
                          rec={en: [] for en in engs}, regs=regs, thresh=thresh)

    def cond_end(self):
        st = self._cond
        self._cond = None
        for en in st["engs"]:
            e = self.E[en]
            d = e.count - st["counts"][en]
            dkeys = [k for k, qn in st["dma_eng"].items() if qn == en and self.dsem[k][1] > st["dcounts"].get(k, 0)]
            if d == 0 and not dkeys and not st["rec"][en]:
                continue
            g = e.be.If_lt(st["regs"][en], st["thresh"])
            g.__enter__()
            for go in st["rec"][en]:
                go()
            g.__exit__(None, None, None)
            g2 = e.be.Else()
            g2.__enter__()
            if d > 0:
                if st["counts"][en] > 0:
                    e.be.wait_ge(e.sem, st["counts"][en])
                e.be.sem_inc(e.sem, d)
            for key in dkeys:
                c0 = st["dcounts"].get(key, 0)
                c1 = self.dsem[key][1]
                if c0 > 0:
                    e.be.wait_ge(self.dsem[key][0], c0)
                e.be.sem_inc(self.dsem[key][0], c1 - c0)
            g2.__exit__(None, None, None)
            e.waited = st["waited"][en]

    def barrier(self):
        evs = [(id(e.sem), e.count) for e in self.E.values() if e.count > 0]
        evs += [(id(s), c) for (s, c) in self.dsem.values() if c > 0]
        for e in self.E.values():
            for k, v in evs:
                if k == id(e.sem) and e.name in ("pe", "pool", "sp"):
                    continue
                if e.waited.get(k, 0) >= v:
                    continue
                e.waited[k] = v
                e.be.wait_ge(self.semobj[k], v)

    def wait_all(self, en, trks):
        e = self.E[en]
        for so, v in self._waits(e, trks, (), True):
            e.be.wait_ge(so, v)


class Builder:
    def __init__(self, ns=NS, nlayers=2, debug=None, moe_experts=NE):
        self.ns = ns
        self.nlayers = nlayers
        self.debug = debug or ()
        self.moe_experts = moe_experts
        self.nc = bass.Bass("TRN2", target_bir_lowering=False)
        self.es = ExitStack()
        self.P = Prog(self.nc, self.es)
        self.cur = self.es
        self.dram = {}
        self.cregs = {en: self.es.enter_context(self.P.E[en].be.register("creg_" + en)) for en in ("pe", "act", "dve", "sp")}

    def din(self, name, shape, dt=F32):
        t = self.nc.dram_tensor(name, list(shape), dt, kind="ExternalInput").ap()
        self.dram[name] = t
        return t

    def dout(self, name, shape, dt=F32):
        t = self.nc.dram_tensor(name, list(shape), dt, kind="ExternalOutput").ap()
        self.dram[name] = t
        return t

    def sb(self, name, shape, dt=F32):
        self._nm = getattr(self, "_nm", 0) + 1
        return self.cur.enter_context(self.nc.sbuf_tensor(f"sb{self._nm}_" + name, list(shape), dt))

    def dump2d(self, dst, src, n, trks):
        P = self.P
        for c0 in range(0, n, 1024):
            w_ = min(1024, n - c0)
            P.op("dve", lambda e, c0=c0, w_=w_: e.tensor_copy(out=self.dbg_stage[:, 0:w_], in_=src[:, c0:c0 + w_]),
                 r=list(trks), w=[self.dbg_stage_t])
            P.dma("sp", dst[:, c0:c0 + w_], self.dbg_stage[:, 0:w_], r=[self.dbg_stage_t], w=[Trk()], key="dbg")

    def alloc_hT(self):
        self.hT = self.sb("hT", [128, 8, 2304], BF16)
        self.hT_t = [Trk() for _ in range(5)]

    def phase(self):
        b = self

        class _Ph:
            def __enter__(s_):
                s_.prev = b.cur
                s_.st = ExitStack()
                b.cur = s_.st
                return s_

            def __exit__(s_, *a):
                b.P.barrier()
                s_.st.close()
                b.cur = s_.prev
                return False

        return _Ph()

    def build(self):
        nc, P = self.nc, self.P
        ns = self.ns
        x_d = self.din("x", [ns, SEQ, D])
        ctx_d = self.din("ctx", [ns, CTX, D])
        cT_d = self.din("cT", [128, 8, 5])
        ada_w_d = self.din("ada_w", [2, D, 6 * D])
        ada_bT_d = self.din("ada_bT", [2, 128, 48])
        n1g_d = self.din("n1gT", [2, 128, 8])
        n2g_d = self.din("n2gT", [2, 128, 8])
        fng_d = self.din("fng", [D])
        ident_d = self.din("ident", [128, 128])
        out_d = self.dout("out", [ns, SEQ, D])
        router_w_d = self.din("router_w", [2, D, NE])
        router_b_d = self.din("router_b", [2, NE])
        bguT_d = self.din("bguT", [2, 128, NE, 16])
        expert_b_down_d = self.din("expert_b_down", [2, NE, D])
        expert_w_gu_d = self.din("expert_w_gu", [2, NE, D, 2 * D])
        expert_w_down_d = self.din("expert_w_down", [2, NE, D, D])
        self.dbg_layer = 0
        mconst_d = self.din("mconst", [128, 192])
        self.G_d = self.nc.dram_tensor("Gscr", [NE * 2560, D], BF16).ap()
        self.Y_d = [self.nc.dram_tensor(f"Yscr{i}", [NE * 2560, 512], F32).ap() for i in range(2)]
        self.G_t = Trk()
        self.Y_t = Trk()
        if "route" in self.debug:
            dbg_dest = self.dout("dbg_dest", [128, 72])
            dbg_gk = self.dout("dbg_gk", [128, 72])
        na_w_qkv_d = self.din("na_w_qkv", [D, 3 * D])
        gla_w_in_d = self.din("gla_w_in", [D, W_IN_A])
        gla_w_out_d = self.din("gla_w_out", [D, D])
        gconst_d = self.din("gconst", [128, 772])
        convp_d = self.din("convp", [128, 4, 34])
        wabd_d = self.din("wabd", [33, 512])
        gla_norm_g_d = self.din("gla_norm_g", [512])
        ropeC_d = self.din("ropeC", [128, 18, 64])
        ropeS_d = self.din("ropeS", [128, 18, 64])
        na_w_out_d = self.din("na_w_out", [D, D])
        na_bt_d = self.din("na_bt", [16, 128, 5, 640])
        if "gates" in self.debug:
            dbg_gates = self.dout("dbg_gates", [128, 18 * NE])
        if "xs" in self.debug:
            dbg_xs = self.dout("dbg_xs", [128, 18, D])
        if "hT" in self.debug:
            dbg_hT = self.dout("dbg_hT", [128, 8, 2304])
        if "mod" in self.debug:
            dbg_mod = self.dout("dbg_mod", [128, 2, 48, 5])

        xs = self.sb("xs", [128, 18, D])
        xs_t = [Trk() for _ in range(18)]
        ps = self.es.enter_context(nc.psum_tensor("ps", [128, 8, 512], F32))
        ps_t = [Trk() for _ in range(8)]
        identf = self.sb("identf", [128, 128])
        identb = self.sb("identb", [128, 128], BF16)
        cst_t = Trk()
        epsc = self.sb("epsc", [128, 1])
        c11 = self.sb("c11", [128, 1])
        onesf = self.sb("onesf", [128, 128])
        onesm = self.sb("onesm", [128, 128])
        modT = self.sb("modT", [128, 2, 48, 5])
        mod_t = Trk()
        A1 = self.sb("A1", [128, 2, 2, 8, 5])
        A_t = Trk()
        ng = self.sb("ng", [128, 2, 2, 8])
        self.bc_diag = [self.sb(f"bc_diag{i}", [128, 128]) for i in range(2)]
        self.bc_diag_t = [Trk(), Trk()]
        self.bc_it = 0
        if self.debug:
            self.dbg_stage = self.sb("dbg_stage", [128, 1024])
            self.dbg_stage_t = Trk()
        self.__dict__.update(locals())

        P.dma("sp", identf[:], ident_d, w=[cst_t], key="c0")
        P.op("dve", lambda e: e.tensor_copy(out=identb[:], in_=identf[:]), r=[cst_t], w=[cst_t])
        P.op("dve", lambda e: e.memset(epsc[:], EPS), w=[cst_t])
        P.op("dve", lambda e: e.memset(c11[:], 7.0 * 1.702), w=[cst_t])
        P.op("dve", lambda e: e.memset(onesf[:], 1.0), w=[cst_t])
        P.op("dve", lambda e: e.memset(onesm[:], 1.0 / 512), w=[cst_t])
        ng_t = Trk()
        for l in range(2):
            P.dma("sp", ng[:, l, 0, :], n1g_d[l], w=[ng_t], key="c0")
            P.dma("sp", ng[:, l, 1, :], n2g_d[l], w=[ng_t], key="c0")
        self.ng_t = ng_t

        with self.phase():
            self.compute_mod()
        if "mod" in self.debug:
            P.dma("sp", dbg_mod, modT[:], r=[mod_t], w=[Trk()], key="dbg")

        self.out_trks = []
        for b in range(ns):
            if "nosample" in self.debug:
                break
            self.sample(b)

        P.wait_all("sp", self.out_trks)
        return nc

    def compute_mod(self):
        nc, P = self.nc, self.P
        cT = self.sb("cT", [128, 8, 5])
        scT = self.sb("scT", [128, 8, 5])
        abT = self.sb("abT", [128, 2, 48])
        c_t = Trk()
        P.dma("sp", cT[:], self.cT_d, w=[c_t], key="c0")
        P.dma("sp", abT[:, 0, :], self.ada_bT_d[0], w=[c_t], key="c0")
        P.dma("sp", abT[:, 1, :], self.ada_bT_d[1], w=[c_t], key="c0")
        P.op("act", lambda e: e.activation(out=scT[:], in_=cT[:], func=AF.Silu), r=[c_t], w=[c_t])
        wst = [self.sb(f"adaw{i}", [128, 8, 512]) for i in range(2)]
        wst_t = [Trk(), Trk()]
        modT, mod_t, ps, ps_t = self.modT, self.mod_t, self.ps, self.ps_t
        wv = self.ada_w_d.rearrange("l (kc p) f -> l p kc f", p=128)
        it = 0
        for l in range(2):
            pbank = ps[:, l, :]
            for cb in range(12):
                buf = it % 2
                q = "sp" if it % 2 == 0 else "act"
                P.dma(q, wst[buf][:], wv[l, :, :, cb * 512:(cb + 1) * 512], w=[wst_t[buf]], key=f"adaw{buf}")
                for jj in range(4):
                    j = cb * 4 + jj
                    for kc in range(8):
                        P.op("pe", lambda e, buf=buf, kc=kc, jj=jj, j=j, pbank=pbank: e.matmul(
                            pbank[:, j * 5:(j + 1) * 5], wst[buf][:, kc, jj * 128:(jj + 1) * 128], scT[:, kc, :],
                            start=(kc == 0), stop=(kc == 7)), r=[wst_t[buf], c_t], w=[ps_t[l]])
                it += 1
            P.op("dve", lambda e, l=l, pbank=pbank: e.tensor_tensor(
                out=modT[:, l, :, :], in0=pbank[:, 0:240].rearrange("p (j b) -> p j b", b=5),
                in1=abT[:, l, :].unsqueeze(2).to_broadcast([128, 48, 5]), op=ALU.add),
                r=[ps_t[l], c_t], w=[mod_t])
        for l in range(2):
            for wh, grp in ((0, 1), (1, 4)):
                P.op("dve", lambda e, l=l, wh=wh, grp=grp: e.scalar_tensor_tensor(
                    out=self.A1[:, l, wh, :, :], in0=modT[:, l, grp * 8:(grp + 1) * 8, :], scalar=1.0,
                    in1=self.ng[:, l, wh, :].unsqueeze(2).to_broadcast([128, 8, 5]),
                    op0=ALU.add, op1=ALU.mult), r=[mod_t, self.ng_t], w=[self.A_t])

    def prenorm(self, b, l, wh, tiles):
        nc, P = self.nc, self.P
        xs, xs_t, hT, hT_t, ps, ps_t = self.xs, self.xs_t, self.hT, self.hT_t, self.ps, self.ps_t
        shgrp = 0 if wh == 0 else 3
        self.pn_ss = [self.sb(f"pn_ss{i}", [128, 2]) for i in range(2)]
        self.pn_ss_t = [Trk() for _ in range(2)]
        self.pn_xb = [self.sb(f"pn_xb{i}", [128, D], BF16) for i in range(2)]
        self.pn_xb_t = [Trk() for _ in range(2)]
        self.pn_it = 0
        for g0 in range(0, len(tiles), 4):
            grp = tiles[g0:g0 + 4]
            ng_ = len(grp)
            gi = grp[0] // 4
            psb = ps.bitcast(BF16)
            for ti, t in enumerate(grp):
                i = self.pn_it % 2
                self.pn_it += 1
                ss, ss_t, xb, xb_t = self.pn_ss[i], self.pn_ss_t[i], self.pn_xb[i], self.pn_xb_t[i]
                P.op("act", lambda e, t=t, ss=ss: e.activation(
                    out=xb[:], in_=xs[:, t, :], func=AF.Square, accum_out=ss[:, 0:1]),
                    r=[xs_t[t]], w=[xb_t, ss_t])
                if "pn1" in self.debug:
                    continue
                P.op("act", lambda e, ss=ss: e.activation(
                    out=ss[:, 1:2], in_=ss[:, 0:1], func=AF.Sqrt, scale=1.0 / D, bias=self.epsc[:, 0:1]),
                    r=[ss_t, self.cst_t], w=[ss_t])
                P.op("dve", lambda e, ss=ss: e.reciprocal(out=ss[:, 1:2], in_=ss[:, 1:2]),
                     r=[ss_t], w=[ss_t])
                if "pn2" in self.debug:
                    continue
                P.op("dve", lambda e, t=t, ss=ss, xb=xb: e.tensor_scalar(
                    out=xb[:], in0=xs[:, t, :], scalar1=ss[:, 1:2], scalar2=None, op0=ALU.mult),
                    r=[ss_t, xs_t[t]], w=[xb_t])
                if "pn3" in self.debug:
                    continue
                for kc in range(8):
                    bank = 4 + kc // 2
                    off = (kc % 2) * 512 + ti * 128
                    P.op("pe", lambda e, bank=bank, off=off, xb=xb, kc=kc: e.transpose(
                        psb[:, bank, off:off + 128], xb[:, kc * 128:(kc + 1) * 128], self.identb[:]),
                        r=[xb_t, self.cst_t], w=[ps_t[bank]])
            if "pn1" in self.debug or "pn2" in self.debug or "pn3" in self.debug or "pn4" in self.debug:
                continue
            ntok = ng_ * 128
            tok0 = grp[0] * 128
            isctx = grp[0] >= 16
            bcol = 4 if isctx else b
            for kc in range(8):
                bank = 4 + kc // 2
                off = (kc % 2) * 512
                P.op("act", lambda e, bank=bank, off=off, kc=kc, ntok=ntok, tok0=tok0, bcol=bcol: e.activation(
                    out=hT[:, kc, tok0:tok0 + ntok], in_=psb[:, bank, off:off + ntok], func=AF.Identity,
                    scale=self.A1[:, l, wh, kc, bcol:bcol + 1],
                    bias=self.modT[:, l, shgrp * 8 + kc, bcol:bcol + 1]),
                    r=[ps_t[bank], self.A_t, self.mod_t], w=[hT_t[gi]])


    def bcast_cols(self, dst, col_fn, dst_trk):
        P, ps, ps_t = self.P, self.ps, self.ps_t
        for kc in range(8):
            i = self.bc_it % 2
            self.bc_it += 1
            dg, dg_t = self.bc_diag[i], self.bc_diag_t[i]
            P.op("dve", lambda e, dg=dg, kc=kc: e.tensor_scalar(
                out=dg[:], in0=self.identf[:], scalar1=col_fn(kc), scalar2=None, op0=ALU.mult),
                r=[self.cst_t, self.mod_t], w=[dg_t])
            bank = kc // 4
            P.op("pe", lambda e, dg=dg, kc=kc, bank=bank: e.matmul(
                ps[:, bank, (kc % 4) * 128:(kc % 4 + 1) * 128], self.onesf[:], dg[:], start=True, stop=True),
                r=[dg_t, self.cst_t], w=[ps_t[bank]])
        for bank in range(2):
            P.op("act", lambda e, bank=bank: e.activation(
                out=dst[:, bank * 512:(bank + 1) * 512], in_=ps[:, bank, :], func=AF.Copy),
                r=[ps_t[bank]], w=[dst_trk])

    def moe(self, b, l, ntiles):
        nc, P = self.nc, self.P
        xs, xs_t, hT, hT_t, ps, ps_t = self.xs, self.xs_t, self.hT, self.hT_t, self.ps, self.ps_t
        modT = self.modT
        ngroups = (ntiles + 3) // 4
        gwid = [min(512, ntiles * 128 - g * 512) for g in range(ngroups)]
        wr = self.sb("wr", [128, 8, NE], BF16)
        brb = self.sb("brb", [128, NE])
        bgT = self.sb("bgT", [128, NE, 16])
        bd = self.sb("bd", [NE, D])
        tb_t = Trk()
        P.dma("pool", wr[:], self.router_w_d[l].rearrange("(kc p) e -> p kc e", p=128), w=[tb_t], key="mt0")
        P.dma("sp", brb[:], self.router_b_d[l].partition_broadcast(128), w=[tb_t], key="mt1")
        P.dma("sp", bgT[:], self.bguT_d[l], w=[tb_t], key="mt1")
        P.dma("sp", bd[:], self.expert_b_down_d[l], w=[tb_t], key="mt1")
        P.op("dve", lambda e: e.tensor_scalar(out=bgT[:, :, 0:8], in0=bgT[:, :, 0:8], scalar1=-1.0, scalar2=7.0,
                                              op0=ALU.mult, op1=ALU.add), r=[tb_t], w=[tb_t])
        P.op("dve", lambda e: e.tensor_scalar(out=bgT[:, :, 8:16], in0=bgT[:, :, 8:16], scalar1=1.0, scalar2=None,
                                              op0=ALU.add), r=[tb_t], w=[tb_t])
        gbc = self.sb("gbc", [128, 2, D])
        gbc_t = [Trk(), Trk()]
        self.bcast_cols(gbc[:, 0, :], lambda kc: modT[:, l, 40 + kc, b:b + 1], gbc_t[0])
        if ntiles > 16:
            self.bcast_cols(gbc[:, 1, :], lambda kc: modT[:, l, 40 + kc, 4:5], gbc_t[1])
        gates = self.sb("gates", [128, 18, NE])
        gates_s = self.sb("gates_s", [128, 18, NE])
        g_t = [Trk() for _ in range(18)]
        lg = [self.sb(f"lg{i}", [128, NE]) for i in range(2)]
        ex = [self.sb(f"ex{i}", [128, NE]) for i in range(2)]
        mx8 = [self.sb(f"mx{i}", [128, 12]) for i in range(2)]
        gT = [self.sb(f"gT{i}", [NE, 128]) for i in range(2)]
        tmpacc = [self.sb("tmpacc", [128, D])] * 2
        tmpacc_t = [Trk()] * 2
        r_t = [Trk(), Trk()]
        gT_t = [Trk(), Trk()]
        for t in range(ntiles):
            i = t % 2
            gi = t // 4
            bank = i
            P.op("pe", lambda e: e.engine_nop(), r=[], w=[]) if False else None
            for kc in range(8):
                P.op("pe", lambda e, kc=kc, t=t, bank=bank: e.matmul(
                    ps[:, bank, 0:NE], hT[:, kc, t * 128:(t + 1) * 128], wr[:, kc, :], start=(kc == 0), stop=(kc == 7)),
                    r=[hT_t[gi], tb_t], w=[ps_t[bank]])
            P.op("dve", lambda e, i=i, bank=bank: e.tensor_tensor(out=lg[i][:], in0=ps[:, bank, 0:NE], in1=brb[:], op=ALU.add),
                 r=[ps_t[bank], tb_t], w=[r_t[i]])
            P.op("dve", lambda e, i=i: e.max(out=mx8[i][:, 0:8], in_=lg[i][:]), r=[r_t[i]], w=[r_t[i]])
            P.op("dve", lambda e, i=i: e.tensor_scalar(out=mx8[i][:, 8:9], in0=mx8[i][:, 0:1], scalar1=-1.0, scalar2=None,
                                                       op0=ALU.mult), r=[r_t[i]], w=[r_t[i]])
            P.op("act", lambda e, i=i: e.activation(out=ex[i][:], in_=lg[i][:], func=AF.Exp, bias=mx8[i][:, 8:9], scale=1.0),
                 r=[r_t[i]], w=[r_t[i]])
            P.op("dve", lambda e, i=i: e.scalar_tensor_tensor(
                out=ex[i][:], in0=lg[i][:], scalar=mx8[i][:, 3:4], in1=ex[i][:], op0=ALU.is_ge, op1=ALU.mult,
                accum_out=mx8[i][:, 9:10]), r=[r_t[i]], w=[r_t[i]])
            P.op("dve", lambda e, i=i: e.reciprocal(out=mx8[i][:, 10:11], in_=mx8[i][:, 9:10]), r=[r_t[i]], w=[r_t[i]])
            P.op("dve", lambda e, i=i, t=t: e.tensor_scalar(out=gates[:, t, :], in0=ex[i][:], scalar1=mx8[i][:, 10:11],
                                                            scalar2=None, op0=ALU.mult), r=[r_t[i]], w=[g_t[t]])
            P.op("dve", lambda e, i=i, t=t: e.tensor_scalar(out=gates_s[:, t, :], in0=ex[i][:], scalar1=mx8[i][:, 10:11],
                                                            scalar2=1.0 / 1.702, op0=ALU.mult, op1=ALU.mult),
                 r=[r_t[i]], w=[g_t[t]])
            P.op("pe", lambda e, t=t, bank=bank: e.transpose(ps[0:NE, bank, 128:256], gates[:, t, :], self.identf[:]),
                 r=[g_t[t], self.cst_t], w=[ps_t[bank]])
            P.op("act", lambda e, i=i, bank=bank: e.activation(out=gT[i][:], in_=ps[0:NE, bank, 128:256], func=AF.Copy),
                 r=[ps_t[bank]], w=[gT_t[i]])
            for hh in range(2):
                bk = 2 + 2 * i + hh
                P.op("pe", lambda e, i=i, hh=hh, bk=bk: e.matmul(ps[:, bk, :], gT[i][:], bd[:, hh * 512:(hh + 1) * 512],
                                                               start=True, stop=True),
                     r=[gT_t[i], tb_t], w=[ps_t[bk]])
            cx = 1 if t >= 16 else 0
            P.op("dve", lambda e, i=i, cx=cx: e.tensor_tensor(
                out=tmpacc[i][:], in0=ps[:, 2 + 2 * i:4 + 2 * i, :].rearrange("p a n -> p (a n)"), in1=gbc[:, cx, :], op=ALU.mult),
                r=[ps_t[2 + 2 * i], ps_t[3 + 2 * i], gbc_t[cx]], w=[tmpacc_t[i]])
            P.op("dve", lambda e, i=i, t=t: e.tensor_tensor(out=xs[:, t, :], in0=xs[:, t, :], in1=tmpacc[i][:], op=ALU.add),
                 r=[tmpacc_t[i], xs_t[t]], w=[xs_t[t]])
        if "gates" in self.debug and b == 0 and l == self.dbg_layer:
            self.dump2d(self.dbg_gates, gates[:].rearrange("p t e -> p (t e)"), 18 * NE, g_t)

        wg = [self.sb(f"wg{i}", [128, 8, 512], BF16) for i in range(2)]
        wu = [self.sb(f"wu{i}", [128, 8, 512], BF16) for i in range(2)]
        wd = [self.sb(f"wd{i}", [128, 4, D], BF16) for i in range(2)]
        w_t = [Trk(), Trk()]
        actb = [self.sb(f"actb{i}", [128, 4, 512], BF16) for i in range(2)]
        actb_t = [Trk(), Trk()]
        rbuf = [self.sb(f"rbuf{i}", [128, 512]) for i in range(2)]
        slbuf = [self.sb(f"slbuf{i}", [128, 512], BF16) for i in range(2)]
        uabuf = [self.sb(f"uabuf{i}", [128, 512]) for i in range(2)]
        el_t = [Trk(), Trk()]
        wgu_v = self.expert_w_gu_d.rearrange("l e (kc p) f -> l e p kc f", p=128)
        wd_v = self.expert_w_down_d.rearrange("l e (kc p) f -> l e p kc f", p=128)
        nhe = self.moe_experts * 2

        def load_w(he):
            e_, hf = he // 2, he % 2
            i = he % 2
            P.dma("pool", wg[i][:], wgu_v[l, e_, :, :, hf * 512:(hf + 1) * 512], w=[w_t[i]], key=f"wg{i}")
            P.dma("pool", wu[i][:], wgu_v[l, e_, :, :, 1024 + hf * 512:1024 + (hf + 1) * 512], w=[w_t[i]], key=f"wg{i}")
            P.dma("pool", wd[i][:], wd_v[l, e_, :, hf * 4:(hf + 1) * 4, :], w=[w_t[i]], key=f"wg{i}")

        units = [(he, g) for he in range(nhe) for g in range(ngroups)]
        self._fcit = 0

        def gu_step(u, fc):
            he, g = units[u]
            e_, hf = he // 2, he % 2
            wi = he % 2
            gw = gwid[g]
            par = self._fcit % 2
            self._fcit += 1
            bg_, bu_ = 2 * par, 2 * par + 1
            for kc in range(8):
                P.op("pe", lambda e, kc=kc: e.matmul(ps[:, bg_, 0:gw], wg[wi][:, kc, fc * 128:(fc + 1) * 128],
                                                    hT[:, kc, g * 512:g * 512 + gw], start=(kc == 0), stop=(kc == 7)),
                     r=[w_t[wi], hT_t[g]], w=[ps_t[bg_]])
            for kc in range(8):
                P.op("pe", lambda e, kc=kc: e.matmul(ps[:, bu_, 0:gw], wu[wi][:, kc, fc * 128:(fc + 1) * 128],
                                                    hT[:, kc, g * 512:g * 512 + gw], start=(kc == 0), stop=(kc == 7)),
                     r=[w_t[wi], hT_t[g]], w=[ps_t[bu_]])
            cg = hf * 4 + fc
            P.op("act", lambda e: e.activation(out=rbuf[par][:, 0:gw], in_=ps[:, bg_, 0:gw], func=AF.Relu,
                                               scale=-1.0, bias=bgT[:, e_, cg:cg + 1]),
                 r=[ps_t[bg_], tb_t], w=[el_t[par]])
            P.op("act", lambda e: e.activation(out=slbuf[par][:, 0:gw], in_=rbuf[par][:, 0:gw], func=AF.Silu,
                                               scale=-1.702, bias=self.c11[:, 0:1]),
                 r=[el_t[par], self.cst_t], w=[el_t[par]])
            P.op("dve", lambda e: e.tensor_scalar(out=uabuf[par][:, 0:gw], in0=ps[:, bu_, 0:gw],
                                                  scalar1=bgT[:, e_, 8 + cg:9 + cg], scalar2=8.0, op0=ALU.add, op1=ALU.min),
                 r=[ps_t[bu_], tb_t], w=[el_t[par]])
            P.op("dve", lambda e: e.scalar_tensor_tensor(out=actb[u % 2][:, fc, 0:gw], in0=uabuf[par][:, 0:gw], scalar=-6.0,
                                                         in1=slbuf[par][:, 0:gw], op0=ALU.max, op1=ALU.mult),
                 r=[el_t[par]], w=[actb_t[u % 2]])

        self._dit = 0

        def down(u):
            he, g = units[u]
            e_, hf = he // 2, he % 2
            wi = he % 2
            nt = gwid[g] // 128
            for tt in range(nt):
                t = g * 4 + tt
                dp = self._dit % 2
                self._dit += 1
                for hh in range(2):
                    bk = 4 + 2 * dp + hh
                    for fc in range(4):
                        P.op("pe", lambda e, fc=fc, bk=bk, hh=hh: e.matmul(
                            ps[:, bk, :], actb[u % 2][:, fc, tt * 128:(tt + 1) * 128], wd[wi][:, fc, hh * 512:(hh + 1) * 512],
                            start=(fc == 0), stop=(fc == 3)), r=[actb_t[u % 2], w_t[wi]], w=[ps_t[bk]])
                cx = 1 if t >= 16 else 0
                P.op("dve", lambda e, dp=dp, cx=cx: e.tensor_tensor(
                    out=tmpacc[dp][:], in0=ps[:, 4 + 2 * dp:6 + 2 * dp, :].rearrange("p a n -> p (a n)"), in1=gbc[:, cx, :],
                    op=ALU.mult), r=[ps_t[4 + 2 * dp], ps_t[5 + 2 * dp], gbc_t[cx]], w=[tmpacc_t[dp]])
                P.op("dve", lambda e, dp=dp, t=t: e.scalar_tensor_tensor(
                    out=xs[:, t, :], in0=tmpacc[dp][:], scalar=gates_s[:, t, e_:e_ + 1], in1=xs[:, t, :],
                    op0=ALU.mult, op1=ALU.add), r=[tmpacc_t[dp], g_t[t], xs_t[t]], w=[xs_t[t]])

        load_w(0)
        if nhe > 1:
            load_w(1)
        for fc in range(4):
            gu_step(0, fc)
        for u in range(len(units)):
            he, g = units[u]
            if u + 1 < len(units):
                gu_step(u + 1, 0)
            down(u)
            if g == ngroups - 1 and he + 2 < nhe:
                load_w(he + 2)
            if u + 1 < len(units):
                for fc in range(1, 4):
                    gu_step(u + 1, fc)


    def moe2(self, b, l, ntiles):
        nc, P = self.nc, self.P
        xs, xs_t, ps, ps_t = self.xs, self.xs_t, self.ps, self.ps_t
        psb = ps.bitcast(BF16)
        modT = self.modT
        CAP = 512
        RS = 2560
        ngr = 5 if ntiles > 16 else 4
        tiles = list(range(ntiles))
        G_d, Y_d = self.G_d, self.Y_d
        gates_s = self.sb("gates_s", [128, 18, NE])
        g_t = [Trk() for _ in range(18)]
        gk = self.sb("gk", [128, 18, 4])
        destf = self.sb("destf", [128, 18, 4])
        destu = self.sb("destu", [128, 18, 4], mybir.dt.uint32)
        rt_t = [Trk() for _ in range(18)]
        cntf = self.sb("cntf", [128, NE])
        cnti = self.sb("cnti", [128, NE], mybir.dt.int32)
        flag_t = Trk()
        bgT = self.sb("bgT", [128, NE, 16])
        tb_t = Trk()
        gbc = self.sb("gbc", [128, 2, D])
        gbc_t = [Trk(), Trk()]
        P.dma("sp", bgT[:], self.bguT_d[l], w=[tb_t], key="mt1")
        P.op("dve", lambda e: e.tensor_scalar(out=bgT[:, :, 0:8], in0=bgT[:, :, 0:8], scalar1=-1.0, scalar2=7.0,
                                              op0=ALU.mult, op1=ALU.add), r=[tb_t], w=[tb_t])
        P.op("dve", lambda e: e.tensor_scalar(out=bgT[:, :, 8:16], in0=bgT[:, :, 8:16], scalar1=1.0, scalar2=None,
                                              op0=ALU.add), r=[tb_t], w=[tb_t])
        self.bcast_cols(gbc[:, 0, :], lambda kc: modT[:, l, 40 + kc, b:b + 1], gbc_t[0])
        if ntiles > 16:
            self.bcast_cols(gbc[:, 1, :], lambda kc: modT[:, l, 40 + kc, 4:5], gbc_t[1])
        self.m = dict(gates_s=gates_s, g_t=g_t, bgT=bgT, tb_t=tb_t, gbc=gbc, gbc_t=gbc_t)

        with self.phase():
            self.alloc_hT()
            hT, hT_t = self.hT, self.hT_t
            with self.phase():
                self.prenorm(b, l, 1, tiles)
            wr = self.sb("wr", [128, 8, NE], BF16)
            brb = self.sb("brb", [128, NE])
            bd = self.sb("bd", [NE, D])
            mc = self.sb("mc", [128, 192])
            mcb = self.sb("mcb", [128, 256], BF16)
            rt0 = Trk()
            P.dma("pool", wr[:], self.router_w_d[l].rearrange("(kc p) e -> p kc e", p=128), w=[rt0], key="mt0")
            P.dma("sp", brb[:], self.router_b_d[l].partition_broadcast(128), w=[rt0], key="mt1")
            P.dma("sp", bd[:], self.expert_b_down_d[l], w=[rt0], key="mt1")
            P.dma("sp", mc[:], self.mconst_d, w=[rt0], key="mt1")
            P.op("dve", lambda e: e.tensor_copy(out=mcb[:, 0:128], in_=mc[:, 0:128]), r=[rt0], w=[rt0])
            P.op("dve", lambda e: e.memset(mcb[:, 128:256], 1.0), w=[rt0])
            gates = self.sb("gates", [128, 18, NE])
            maskb = self.sb("maskb", [128, 18, NE], BF16)
            mk_t = [Trk() for _ in range(18)]
            lg = [self.sb(f"lg{i}", [128, NE]) for i in range(2)]
            ex = [self.sb(f"ex{i}", [128, NE]) for i in range(2)]
            mx8 = [self.sb(f"mx{i}", [128, 12]) for i in range(2)]
            idxu = [self.sb(f"idxu{i}", [128, 8], mybir.dt.uint32) for i in range(2)]
            idxf = self.sb("idxf", [128, 18, 8])
            gT = [self.sb(f"gT{i}", [NE, 128]) for i in range(2)]
            tmpacc = self.sb("tmpacc", [128, D])
            tmpacc_t = Trk()
            r_t = [Trk(), Trk()]
            gT_t = [Trk(), Trk()]
            for t in tiles:
                i = t % 2
                gi = t // 4
                bank = i
                for kc in range(8):
                    P.op("pe", lambda e, kc=kc, t=t, bank=bank: e.matmul(
                        ps[:, bank, 0:NE], hT[:, kc, t * 128:(t + 1) * 128], wr[:, kc, :], start=(kc == 0), stop=(kc == 7)),
                        r=[hT_t[gi], rt0], w=[ps_t[bank]])
                P.op("dve", lambda e, i=i, bank=bank: e.tensor_tensor(out=lg[i][:], in0=ps[:, bank, 0:NE], in1=brb[:], op=ALU.add),
                     r=[ps_t[bank], rt0], w=[r_t[i]])
                P.op("dve", lambda e, i=i: e.max(out=mx8[i][:, 0:8], in_=lg[i][:]), r=[r_t[i]], w=[r_t[i]])
                P.op("dve", lambda e, i=i: e.max_index(out=idxu[i][:], in_max=mx8[i][:, 0:8], in_values=lg[i][:]), r=[r_t[i]], w=[r_t[i]])
                P.op("dve", lambda e, i=i, t=t: e.tensor_copy(out=idxf[:, t, :], in_=idxu[i][:]), r=[r_t[i]], w=[rt_t[t]])
                P.op("dve", lambda e, i=i: e.tensor_scalar(out=mx8[i][:, 8:9], in0=mx8[i][:, 0:1], scalar1=-1.0, scalar2=None,
                                                           op0=ALU.mult), r=[r_t[i]], w=[r_t[i]])
                P.op("act", lambda e, i=i: e.activation(out=ex[i][:], in_=lg[i][:], func=AF.Exp, bias=mx8[i][:, 8:9], scale=1.0),
                     r=[r_t[i]], w=[r_t[i]])
                P.op("dve", lambda e, i=i, t=t: e.tensor_scalar(out=maskb[:, t, :], in0=lg[i][:], scalar1=mx8[i][:, 3:4], scalar2=None,
                                                                op0=ALU.is_ge), r=[r_t[i]], w=[mk_t[t]])
                P.op("dve", lambda e, i=i: e.scalar_tensor_tensor(
                    out=ex[i][:], in0=lg[i][:], scalar=mx8[i][:, 3:4], in1=ex[i][:], op0=ALU.is_ge, op1=ALU.mult,
                    accum_out=mx8[i][:, 9:10]), r=[r_t[i]], w=[r_t[i]])
                P.op("dve", lambda e, i=i: e.reciprocal(out=mx8[i][:, 10:11], in_=mx8[i][:, 9:10]), r=[r_t[i]], w=[r_t[i]])
                P.op("dve", lambda e, i=i, t=t: e.tensor_scalar(out=gates[:, t, :], in0=ex[i][:], scalar1=mx8[i][:, 10:11],
                                                                scalar2=None, op0=ALU.mult), r=[r_t[i]], w=[g_t[t]])
                P.op("dve", lambda e, i=i, t=t: e.tensor_scalar(out=gates_s[:, t, :], in0=ex[i][:], scalar1=mx8[i][:, 10:11],
                                                                scalar2=1.0 / 1.702, op0=ALU.mult, op1=ALU.mult),
                     r=[r_t[i]], w=[g_t[t]])
                P.op("pe", lambda e, t=t, bank=bank: e.transpose(ps[0:NE, bank, 128:256], gates[:, t, :], self.identf[:]),
                     r=[g_t[t], self.cst_t], w=[ps_t[bank]])
                P.op("act", lambda e, i=i, bank=bank: e.activation(out=gT[i][:], in_=ps[0:NE, bank, 128:256], func=AF.Copy),
                     r=[ps_t[bank]], w=[gT_t[i]])
                for hh in range(2):
                    bk = 2 + 2 * i + hh
                    P.op("pe", lambda e, i=i, hh=hh, bk=bk: e.matmul(ps[:, bk, :], gT[i][:], bd[:, hh * 512:(hh + 1) * 512],
                                                                   start=True, stop=True),
                         r=[gT_t[i], rt0], w=[ps_t[bk]])
                cx = 1 if t >= 16 else 0
                self.resid_add(t, 2 + 2 * i, gbc[:, cx, :], gbc_t[cx], tmpacc, tmpacc_t)
            posc = [self.sb(f"posc{i}", [128, NE]) for i in range(2)]
            pjunk = self.sb("pjunk", [128, NE])
            pc_t = [Trk(), Trk()]
            for t in tiles:
                i = t % 2
                bank = 6 + i
                for t2 in range(t + 1):
                    lhs = mcb[:, 0:128] if t2 == t else mcb[:, 128:256]
                    P.op("pe", lambda e, t2=t2, lhs=lhs, bank=bank, t=t: e.matmul(
                        ps[:, bank, 0:NE], lhs, maskb[:, t2, :], start=(t2 == 0), stop=(t2 == t)),
                        r=[mk_t[t2], rt0], w=[ps_t[bank]])
                P.op("dve", lambda e, i=i, bank=bank: e.tensor_tensor(out=posc[i][:], in0=ps[:, bank, 0:NE], in1=mc[:, 128:160], op=ALU.add),
                     r=[ps_t[bank], rt0], w=[pc_t[i]])
                for k in range(4):
                    P.op("dve", lambda e, i=i, t=t, k=k: e.scalar_tensor_tensor(
                        out=pjunk[:], in0=mc[:, 160:192], scalar=idxf[:, t, k:k + 1], in1=posc[i][:], op0=ALU.is_equal, op1=ALU.mult,
                        accum_out=destf[:, t, k:k + 1]), r=[pc_t[i], rt_t[t], rt0], w=[rt_t[t]])
                    P.op("dve", lambda e, i=i, t=t, k=k: e.scalar_tensor_tensor(
                        out=pjunk[:], in0=mc[:, 160:192], scalar=idxf[:, t, k:k + 1], in1=gates_s[:, t, :], op0=ALU.is_equal, op1=ALU.mult,
                        accum_out=gk[:, t, k:k + 1]), r=[g_t[t], rt_t[t], rt0], w=[rt_t[t]])
                P.op("dve", lambda e, t=t: e.tensor_copy(out=destu[:, t, :], in_=destf[:, t, :]), r=[rt_t[t]], w=[rt_t[t]])
            for t2 in tiles:
                P.op("pe", lambda e, t2=t2: e.matmul(ps[:, 5, 0:NE], mcb[:, 128:256], maskb[:, t2, :], start=(t2 == 0), stop=(t2 == ntiles - 1)),
                     r=[mk_t[t2], rt0], w=[ps_t[5]])
            P.op("dve", lambda e: e.tensor_scalar(out=cntf[:], in0=ps[:, 5, 0:NE], scalar1=-1.0, scalar2=4096.0, op0=ALU.mult, op1=ALU.add),
                 r=[ps_t[5]], w=[flag_t])
            P.op("dve", lambda e: e.tensor_copy(out=cnti[:], in_=cntf[:]), r=[flag_t], w=[flag_t])
            if "route" in self.debug and b == 0 and l == self.dbg_layer:
                self.dump2d(self.dbg_dest, destf[:].rearrange("p t k -> p (t k)"), 72, rt_t)
                self.dump2d(self.dbg_gk, gk[:].rearrange("p t k -> p (t k)"), 72, rt_t)
            h2t = [self.sb(f"h2t{i}", [128, D], BF16) for i in range(2)]
            h2t_t = [Trk(), Trk()]
            for t in tiles:
                i = t % 2
                bank = 6 + i
                for kc in range(8):
                    P.op("pe", lambda e, kc=kc, t=t, bank=bank: e.transpose(
                        psb[:, bank, kc * 128:(kc + 1) * 128], hT[:, kc, t * 128:(t + 1) * 128], self.identb[:]),
                        r=[hT_t[t // 4], self.cst_t], w=[ps_t[bank]])
                P.op("act", lambda e, i=i, bank=bank: e.activation(out=h2t[i][:], in_=psb[:, bank, :], func=AF.Copy),
                     r=[ps_t[bank]], w=[h2t_t[i]])
                for k in range(4):
                    self.P.idma("pool", G_d, bass.IndirectOffsetOnAxis(destu[:, t, k:k + 1], 0), h2t[i][:], None,
                                r=[h2t_t[i], rt_t[t]], w=[self.G_t], key=f"sc{(t * 4 + k) % 8}")

        with self.phase():
            wg = [self.sb(f"wg{i}", [128, 8, 512], BF16) for i in range(2)]
            wu = [self.sb(f"wu{i}", [128, 8, 512], BF16) for i in range(2)]
            wd = self.sb("wd_single", [128, 8, D], BF16)
            w_t = [Trk(), Trk()]
            wd_t = Trk()
            Gt = self.sb("Gt", [128, 4, D], BF16)
            Gt_t = Trk()
            gTb = self.sb("gTb", [128, 8, 512], BF16)
            gTb_t = Trk()
            actb = self.sb("actb", [128, 8, 512], BF16)
            actb_t = Trk()
            rbuf = [self.sb(f"rbuf{i}", [128, 512]) for i in range(2)]
            slbuf = [self.sb(f"slbuf{i}", [128, 512], BF16) for i in range(2)]
            uabuf = [self.sb(f"uabuf{i}", [128, 512]) for i in range(2)]
            el_t = [Trk(), Trk()]
            yout = [self.sb(f"yout{i}", [128, 512]) for i in range(2)]
            yout_t = [Trk(), Trk()]
            wgu_v = self.expert_w_gu_d.rearrange("l e (kc p) f -> l e p kc f", p=128)
            wd_v = self.expert_w_down_d.rearrange("l e (kc p) f -> l e p kc f", p=128)
            nexp = self.moe_experts
            cengs = ("pe", "act", "dve", "sp")
            for en in cengs:
                P.wait_all(en, [flag_t])

            def load_w(e_, hf):
                P.dma("pool", wg[hf][:], wgu_v[l, e_, :, :, hf * 512:(hf + 1) * 512], w=[w_t[hf]], key=f"wg{hf}")
                P.dma("pool", wu[hf][:], wgu_v[l, e_, :, :, 1024 + hf * 512:1024 + (hf + 1) * 512], w=[w_t[hf]], key=f"wg{hf}")

            self._fcit = 0
            for e_ in range(nexp):
                load_w(e_, 0)
                load_w(e_, 1)
                P.dma("pool", wd[:], wd_v[l, e_], w=[wd_t], key="wd0")
                for en in cengs:
                    P.E[en].be.reg_load(self.cregs[en], cnti[0:1, e_:e_ + 1])
                for gi in range(ngr):
                    P.cond_begin(self.cregs, 4096 - gi * CAP, cengs)
                    row0 = e_ * RS + gi * CAP
                    P.dma("sp", Gt[:], G_d[row0:row0 + CAP, :].rearrange("(a p) f -> p a f", p=128),
                          r=[self.G_t], w=[Gt_t], key="gtl")
                    for kc in range(8):
                        bank = 6 + kc % 2
                        for a in range(4):
                            P.op("pe", lambda e, kc=kc, a=a, bank=bank: e.transpose(
                                psb[:, bank, a * 128:(a + 1) * 128], Gt[:, a, kc * 128:(kc + 1) * 128], self.identb[:]),
                                r=[Gt_t, self.cst_t], w=[ps_t[bank]])
                        P.op("act", lambda e, kc=kc, bank=bank: e.activation(out=gTb[:, kc, :], in_=psb[:, bank, 0:512], func=AF.Copy),
                             r=[ps_t[bank]], w=[gTb_t])
                    for hf in range(2):
                        for fc in range(4):
                            par = self._fcit % 2
                            self._fcit += 1
                            bg_, bu_ = 2 * par, 2 * par + 1
                            for kc in range(8):
                                P.op("pe", lambda e, kc=kc, hf=hf, fc=fc, bg_=bg_: e.matmul(
                                    ps[:, bg_, :], wg[hf][:, kc, fc * 128:(fc + 1) * 128], gTb[:, kc, :],
                                    start=(kc == 0), stop=(kc == 7)), r=[w_t[hf], gTb_t], w=[ps_t[bg_]])
                            for kc in range(8):
                                P.op("pe", lambda e, kc=kc, hf=hf, fc=fc, bu_=bu_: e.matmul(
                                    ps[:, bu_, :], wu[hf][:, kc, fc * 128:(fc + 1) * 128], gTb[:, kc, :],
                                    start=(kc == 0), stop=(kc == 7)), r=[w_t[hf], gTb_t], w=[ps_t[bu_]])
                            cg = hf * 4 + fc
                            P.op("act", lambda e, par=par, bg_=bg_, cg=cg: e.activation(
                                out=rbuf[par][:], in_=ps[:, bg_, :], func=AF.Relu, scale=-1.0, bias=bgT[:, e_, cg:cg + 1]),
                                r=[ps_t[bg_], tb_t], w=[el_t[par]])
                            P.op("act", lambda e, par=par: e.activation(
                                out=slbuf[par][:], in_=rbuf[par][:], func=AF.Silu, scale=-1.702, bias=self.c11[:, 0:1]),
                                r=[el_t[par], self.cst_t], w=[el_t[par]])
                            P.op("dve", lambda e, par=par, bu_=bu_, cg=cg: e.tensor_scalar(
                                out=uabuf[par][:], in0=ps[:, bu_, :], scalar1=bgT[:, e_, 8 + cg:9 + cg], scalar2=8.0,
                                op0=ALU.add, op1=ALU.min), r=[ps_t[bu_], tb_t], w=[el_t[par]])
                            P.op("dve", lambda e, par=par, cg=cg: e.scalar_tensor_tensor(
                                out=actb[:, cg, :], in0=uabuf[par][:], scalar=-6.0, in1=slbuf[par][:],
                                op0=ALU.max, op1=ALU.mult), r=[el_t[par]], w=[actb_t])
                    for a in range(4):
                        for hh in range(2):
                            bk = 4 + hh
                            for c in range(8):
                                P.op("pe", lambda e, c=c, bk=bk, hh=hh, a=a: e.matmul(
                                    ps[:, bk, :], actb[:, c, a * 128:(a + 1) * 128], wd[:, c, hh * 512:(hh + 1) * 512],
                                    start=(c == 0), stop=(c == 7)), r=[actb_t, wd_t], w=[ps_t[bk]])
                            P.op("act", lambda e, bk=bk, hh=hh: e.activation(out=yout[hh][:], in_=ps[:, bk, :], func=AF.Copy),
                                 r=[ps_t[bk]], w=[yout_t[hh]])
                            P.dma("sp", Y_d[hh][row0 + a * 128:row0 + (a + 1) * 128, :], yout[hh][:],
                                  r=[yout_t[hh]], w=[self.Y_t], key=f"yo{hh}")
                    P.cond_end()

        with self.phase():
            yk = [self.sb(f"yk{i}", [128, D]) for i in range(4)]
            yk_t = [Trk() for _ in range(4)]
            facc = self.sb("facc", [128, D])
            facc_t = Trk()
            it = 0
            for t in tiles:
                for k in range(4):
                    i = it % 4
                    it += 1
                    for hh in range(2):
                        self.P.idma("pool", yk[i][:, hh * 512:(hh + 1) * 512], None, Y_d[hh], bass.IndirectOffsetOnAxis(destu[:, t, k:k + 1], 0),
                                    r=[self.Y_t, rt_t[t]], w=[yk_t[i]], key=f"ga{i}")
                    if k == 0:
                        P.op("dve", lambda e, i=i, t=t, k=k: e.tensor_scalar(out=facc[:], in0=yk[i][:], scalar1=gk[:, t, k:k + 1], scalar2=None,
                                                                            op0=ALU.mult), r=[yk_t[i], rt_t[t]], w=[facc_t])
                    else:
                        P.op("dve", lambda e, i=i, t=t, k=k: e.scalar_tensor_tensor(out=facc[:], in0=yk[i][:], scalar=gk[:, t, k:k + 1], in1=facc[:],
                                                                                   op0=ALU.mult, op1=ALU.add), r=[yk_t[i], rt_t[t], facc_t], w=[facc_t])
                cx = 1 if t >= 16 else 0
                P.op("dve", lambda e, cx=cx: e.tensor_tensor(out=facc[:], in0=facc[:], in1=gbc[:, cx, :], op=ALU.mult),
                     r=[facc_t, gbc_t[cx]], w=[facc_t])
                P.op("dve", lambda e, t=t: e.tensor_tensor(out=xs[:, t, :], in0=xs[:, t, :], in1=facc[:], op=ALU.add),
                     r=[facc_t, xs_t[t]], w=[xs_t[t]])

    def resid_add(self, t, bank0, gbc_ap, gbc_trk, tmp, tmp_t):
        P, ps, ps_t, xs, xs_t = self.P, self.ps, self.ps_t, self.xs, self.xs_t
        P.op("dve", lambda e: e.tensor_tensor(
            out=tmp[:], in0=ps[:, bank0:bank0 + 2, :].rearrange("p a n -> p (a n)"), in1=gbc_ap, op=ALU.mult),
            r=[ps_t[bank0], ps_t[bank0 + 1], gbc_trk], w=[tmp_t])
        P.op("dve", lambda e: e.tensor_tensor(out=xs[:, t, :], in0=xs[:, t, :], in1=tmp[:], op=ALU.add),
             r=[tmp_t, xs_t[t]], w=[xs_t[t]])

    def mixer_na(self, b, l):
        nc, P = self.nc, self.P
        xs, xs_t, hT, hT_t, ps, ps_t = self.xs, self.xs_t, self.hT, self.hT_t, self.ps, self.ps_t
        psb = ps.bitcast(BF16)
        oT = self.sb("oT", [128, 8, SEQ], BF16)
        oT_t = [[Trk() for _ in range(4)] for _ in range(8)]
        wq_v = self.na_w_qkv_d.rearrange("(kc p) f -> p kc f", p=128)
        with self.phase():
            wq = self.sb("wq", [128, 8, 128], BF16)
            wk = self.sb("wk", [128, 8, 128], BF16)
            wv = self.sb("wv", [128, 8, 128], BF16)
            w_t = Trk()
            qT = self.sb("qT", [128, SEQ], BF16)
            kT = self.sb("kT", [128, 2304], BF16)
            vA = self.sb("vA", [128, 18, 2, 65], BF16)
            otok = self.sb("otok", [128, 16, 128], BF16)
            q_t, k_t, v_t, otok_t = Trk(), Trk(), Trk(), Trk()
            BT = self.sb("BT", [128, 5, 640])
            BT_t = Trk()
            sbt = [self.sb(f"sbt{i}", [128, 640]) for i in range(2)]
            sbt_t = [Trk(), Trk()]
            PT = [self.sb(f"PT{i}", [128, 896], BF16) for i in range(2)]
            PT_t = [Trk(), Trk()]
            rden = [self.sb(f"rden{i}", [128, 1]) for i in range(2)]
            rden_t = [Trk(), Trk()]
            P.op("dve", lambda e: e.memset(vA[:], 1.0), w=[v_t])
            for hp in range(8):
                P.dma("pool", wq[:], wq_v[:, :, hp * 128:(hp + 1) * 128], w=[w_t], key="naw")
                P.dma("pool", wk[:], wq_v[:, :, 1024 + hp * 128:1024 + (hp + 1) * 128], w=[w_t], key="naw")
                P.dma("pool", wv[:], wq_v[:, :, 2048 + hp * 128:2048 + (hp + 1) * 128], w=[w_t], key="naw")
                it = 0
                for g in range(5):
                    gw = 512 if g < 4 else 256
                    for which in range(2):
                        if which == 0 and g == 4:
                            continue
                        bank = 6 + it % 2
                        it += 1
                        wsrc = wq if which == 0 else wk
                        for kc in range(8):
                            P.op("pe", lambda e, kc=kc, bank=bank, wsrc=wsrc, g=g, gw=gw: e.matmul(
                                ps[:, bank, 0:gw], wsrc[:, kc, :], hT[:, kc, g * 512:g * 512 + gw],
                                start=(kc == 0), stop=(kc == 7)), r=[w_t, hT_t[g]], w=[ps_t[bank]])
                        if which == 0:
                            P.op("act", lambda e, bank=bank, g=g, gw=gw: e.activation(
                                out=qT[:, g * 512:g * 512 + gw], in_=ps[:, bank, 0:gw], func=AF.Copy, scale=0.125),
                                r=[ps_t[bank]], w=[q_t])
                        else:
                            P.op("act", lambda e, bank=bank, g=g, gw=gw: e.activation(
                                out=kT[:, g * 512:g * 512 + gw], in_=ps[:, bank, 0:gw], func=AF.Copy),
                                r=[ps_t[bank]], w=[k_t])
                for t in range(18):
                    bank = 6 + t % 2
                    for kc in range(8):
                        P.op("pe", lambda e, kc=kc, bank=bank, t=t: e.matmul(
                            ps[:, bank, 0:128], hT[:, kc, t * 128:(t + 1) * 128], wv[:, kc, :],
                            start=(kc == 0), stop=(kc == 7)), r=[w_t, hT_t[t // 4]], w=[ps_t[bank]])
                    P.op("dve", lambda e, bank=bank, t=t: e.tensor_copy(
                        out=vA[:, t, :, 0:64], in_=ps[:, bank, 0:128].rearrange("p (h d) -> p h d", h=2)),
                        r=[ps_t[bank]], w=[v_t])
                units = [(h, p) for h in range(2) for p in range(16)]

                def st_step(u):
                    h, p = units[u]
                    i = u % 2
                    if p == 0:
                        head = hp * 2 + h
                        P.dma("sp", BT[:], self.na_bt_d[head], w=[BT_t], key="nabt")
                    Bp = min(max(2 * p - 4, 0), 22)
                    hr = slice(h * 64, (h + 1) * 64)
                    bA = 2 * i
                    for j in range(5):
                        tok = (Bp + 2 * j) * 64
                        dst = ps[:, bA, j * 128:(j + 1) * 128] if j < 4 else ps[:, bA + 1, 0:128]
                        P.op("pe", lambda e, dst=dst, tok=tok: e.matmul(
                            dst, kT[hr, tok:tok + 128], qT[hr, p * 128:(p + 1) * 128], start=True, stop=True),
                            r=[q_t, k_t], w=[ps_t[bA if j < 4 else bA + 1]])
                    for c in range(2):
                        P.op("pe", lambda e, c=c: e.matmul(
                            ps[:, bA + 1, 128 + c * 128:256 + c * 128], kT[hr, 2048 + c * 128:2176 + c * 128],
                            qT[hr, p * 128:(p + 1) * 128], start=True, stop=True), r=[q_t, k_t], w=[ps_t[bA + 1]])
                    var = {0: 0, 1: 1, 14: 3, 15: 4}.get(p, 2)
                    flat = ps[:, bA:bA + 2, :].rearrange("p a n -> p (a n)")
                    P.op("dve", lambda e: e.tensor_tensor(out=sbt[i][:], in0=flat[:, 0:640], in1=BT[:, var, :], op=ALU.add),
                         r=[ps_t[bA], ps_t[bA + 1], BT_t], w=[sbt_t[i]])
                    P.op("act", lambda e: e.activation(out=PT[i][:, 0:640], in_=sbt[i][:], func=AF.Exp),
                         r=[sbt_t[i]], w=[PT_t[i]])
                    P.op("act", lambda e: e.activation(out=PT[i][:, 640:896], in_=flat[:, 640:896], func=AF.Exp),
                         r=[ps_t[bA + 1]], w=[PT_t[i]])

                def pv_step(u):
                    h, p = units[u]
                    i = u % 2
                    Bp = min(max(2 * p - 4, 0), 22)
                    bO = 4 + i
                    tiles = [Bp // 2 + j for j in range(5)] + [16, 17]
                    for ci, tl in enumerate(tiles):
                        P.op("pe", lambda e, ci=ci, tl=tl: e.matmul(
                            ps[:, bO, 0:65], PT[i][:, ci * 128:(ci + 1) * 128], vA[:, tl, h, :],
                            start=(ci == 0), stop=(ci == 6)), r=[PT_t[i], v_t], w=[ps_t[bO]])
                    P.op("dve", lambda e: e.reciprocal(out=rden[i][:], in_=ps[:, bO, 64:65]), r=[ps_t[bO]], w=[rden_t[i]])
                    P.op("dve", lambda e: e.tensor_scalar(out=otok[:, p, h * 64:(h + 1) * 64], in0=ps[:, bO, 0:64],
                                                          scalar1=rden[i][:, 0:1], scalar2=None, op0=ALU.mult),
                         r=[ps_t[bO], rden_t[i]], w=[otok_t])

                st_step(0)
                for u in range(len(units)):
                    if u + 1 < len(units):
                        st_step(u + 1)
                    pv_step(u)
                for g in range(4):
                    bank = 6 + g % 2
                    for tt in range(4):
                        t = g * 4 + tt
                        P.op("pe", lambda e, bank=bank, tt=tt, t=t: e.transpose(
                            psb[:, bank, tt * 128:(tt + 1) * 128], otok[:, t, :], self.identb[:]),
                            r=[otok_t, self.cst_t], w=[ps_t[bank]])
                    P.op("act", lambda e, bank=bank, g=g: e.activation(
                        out=oT[:, hp, g * 512:(g + 1) * 512], in_=psb[:, bank, 0:512], func=AF.Copy),
                        r=[ps_t[bank]], w=[oT_t[hp][g]])
        with self.phase():
            wo = self.sb("wo", [128, 8, D], BF16)
            wo_t = Trk()
            P.dma("pool", wo[:], self.na_w_out_d.rearrange("(kc p) f -> p kc f", p=128), w=[wo_t], key="naw")
            gbc = self.sb("gbc1", [128, D])
            gbc_t = Trk()
            tmp = self.sb("tmp1", [128, D])
            tmp_t = Trk()
            self.bcast_cols(gbc[:], lambda kc: self.modT[:, l, 16 + kc, b:b + 1], gbc_t)
            for t in range(16):
                b0 = 4 + 2 * (t % 2)
                for hh in range(2):
                    for hp in range(8):
                        P.op("pe", lambda e, hh=hh, hp=hp, t=t, b0=b0: e.matmul(
                            ps[:, b0 + hh, :], oT[:, hp, t * 128:(t + 1) * 128], wo[:, hp, hh * 512:(hh + 1) * 512],
                            start=(hp == 0), stop=(hp == 7)), r=[oT_t[hp][t // 4], wo_t], w=[ps_t[b0 + hh]])
                self.resid_add(t, b0, gbc[:], gbc_t, tmp, tmp_t)


    def mixer_gla(self, b, l):
        nc, P = self.nc, self.P
        xs, xs_t, hT, hT_t, ps, ps_t = self.xs, self.xs_t, self.hT, self.hT_t, self.ps, self.ps_t
        psb = ps.bitcast(BF16)
        modT = self.modT
        win_v = self.gla_w_in_d.rearrange("(kc p) f -> p kc f", p=128)
        wout_v = self.gla_w_out_d.rearrange("(kc p) f -> p kc f", p=128)
        gwid = [512, 512, 512, 512, 256]
        gbc = self.sb("gbcA", [128, 2, D])
        gbc_t = [Trk(), Trk()]
        tmp = self.sb("tmpA", [128, D])
        tmp_t = Trk()
        self.bcast_cols(gbc[:, 0, :], lambda kc: modT[:, l, 16 + kc, b:b + 1], gbc_t[0])
        self.bcast_cols(gbc[:, 1, :], lambda kc: modT[:, l, 16 + kc, 4:5], gbc_t[1])
        gc = self.sb("gconst", [128, 772])
        gc_t = Trk()
        P.dma("sp", gc[:], self.gconst_d, w=[gc_t], key="gc")
        McF, McB, MsF, MsB = gc[:, 0:128], gc[:, 128:256], gc[:, 256:384], gc[:, 384:512]
        mkF, mkB, Ind = gc[:, 512:640], gc[:, 640:768], gc[:, 768:770]

        with self.phase():
            uT = self.sb("uT", [128, 4, 2078], BF16)
            uTc = self.sb("uTc", [128, 4, 286], BF16)
            u_t = Trk()
            P.op("dve", lambda e: e.memset(uT[:], 0.0), w=[u_t])
            P.op("dve", lambda e: e.memset(uTc[:], 0.0), w=[u_t])
            cvp = self.sb("cvp", [128, 4, 34])
            cvp_t = Trk()
            P.dma("sp", cvp[:], self.convp_d, w=[cvp_t], key="gc")
            with self.phase():
                wa = [self.sb(f"wa{i}", [128, 8, 128], BF16) for i in range(2)]
                wg_ = [self.sb(f"wgt{i}", [128, 8, 128], BF16) for i in range(2)]
                wa_t = [Trk(), Trk()]
                sg = [self.sb(f"sg{i}", [128, 512]) for i in range(2)]
                sg_t = [Trk(), Trk()]
                it = 0
                for j in range(4):
                    i = j % 2
                    P.dma("pool", wa[i][:], win_v[:, :, OFF_GLU + j * 128:OFF_GLU + (j + 1) * 128], w=[wa_t[i]], key=f"cw{i}")
                    P.dma("pool", wg_[i][:], win_v[:, :, OFF_GLU + 512 + j * 128:OFF_GLU + 512 + (j + 1) * 128], w=[wa_t[i]], key=f"cw{i}")
                    for g in range(5):
                        gw = gwid[g]
                        k2 = it % 2
                        it += 1
                        ba, bg = 2 * k2, 2 * k2 + 1
                        for kc in range(8):
                            P.op("pe", lambda e, kc=kc, g=g, gw=gw, ba=ba, i=i: e.matmul(
                                ps[:, ba, 0:gw], wa[i][:, kc, :], hT[:, kc, g * 512:g * 512 + gw], start=(kc == 0), stop=(kc == 7)),
                                r=[wa_t[i], hT_t[g]], w=[ps_t[ba]])
                        for kc in range(8):
                            P.op("pe", lambda e, kc=kc, g=g, gw=gw, bg=bg, i=i: e.matmul(
                                ps[:, bg, 0:gw], wg_[i][:, kc, :], hT[:, kc, g * 512:g * 512 + gw], start=(kc == 0), stop=(kc == 7)),
                                r=[wa_t[i], hT_t[g]], w=[ps_t[bg]])
                        P.op("act", lambda e, k2=k2, bg=bg, gw=gw: e.activation(out=sg[k2][:, 0:gw], in_=ps[:, bg, 0:gw], func=AF.Sigmoid),
                             r=[ps_t[bg]], w=[sg_t[k2]])
                        dst = uT[:, j, 15 + g * 512:15 + g * 512 + gw] if g < 4 else uTc[:, j, 15:15 + gw]
                        P.op("dve", lambda e, dst=dst, ba=ba, k2=k2, gw=gw: e.tensor_tensor(
                            out=dst, in0=ps[:, ba, 0:gw], in1=sg[k2][:, 0:gw], op=ALU.mult),
                            r=[ps_t[ba], sg_t[k2]], w=[u_t])
            with self.phase():
                NDG = 8
                dg = [self.sb(f"dg{i}", [128, 128], BF16) for i in range(NDG)]
                dg_t = [Trk() for _ in range(NDG)]
                yf = self.sb("yf", [128, 4, 512])
                ysq = self.sb("ysq", [128, 4, 512])
                yf_t = [Trk() for _ in range(4)]
                st1 = self.sb("st1", [128, 512])
                st2 = self.sb("st2", [128, 512])
                st_t = Trk()
                t1 = [self.sb(f"t1_{i}", [128, 512]) for i in range(2)]
                t1_t = [Trk(), Trk()]
                ycT = self.sb("ycT", [128, 4, 512], BF16)
                ycT_t = Trk()
                woc = self.sb("woc", [128, 4, D], BF16)
                woc_t = Trk()
                P.dma("pool", woc[:], wout_v[:, 4:8, :], w=[woc_t], key="cw0")
                dit = 0
                for g in range(5):
                    gw = gwid[g]
                    for j in range(4):
                        src = uT[:, j, :] if g < 4 else uTc[:, j, :]
                        off = g * 512 if g < 4 else 0
                        for tap in range(31):
                            di = dit % NDG
                            dit += 1
                            P.op("dve", lambda e, di=di, j=j, tap=tap: e.tensor_scalar(
                                out=dg[di][:], in0=self.identf[:], scalar1=cvp[:, j, tap:tap + 1], scalar2=None, op0=ALU.mult),
                                r=[self.cst_t, cvp_t], w=[dg_t[di]])
                            P.op("pe", lambda e, di=di, j=j, tap=tap, src=src, off=off, gw=gw: e.matmul(
                                ps[:, j, 0:gw], dg[di][:], src[:, off + tap:off + tap + gw], start=(tap == 0), stop=(tap == 30)),
                                r=[dg_t[di], u_t], w=[ps_t[j]])
                        P.op("act", lambda e, j=j, gw=gw: e.activation(out=yf[:, j, 0:gw], in_=ps[:, j, 0:gw], func=AF.Identity,
                                                                        bias=cvp[:, j, 31:32], scale=1.0),
                             r=[ps_t[j], cvp_t], w=[yf_t[j]])
                        P.op("act", lambda e, j=j, gw=gw: e.activation(out=ysq[:, j, 0:gw], in_=ps[:, j, 0:gw], func=AF.Square,
                                                                        bias=cvp[:, j, 31:32], scale=1.0),
                             r=[ps_t[j], cvp_t], w=[yf_t[j]])
                    for j in range(4):
                        P.op("pe", lambda e, j=j, gw=gw: e.matmul(ps[:, 4, 0:gw], self.onesm[:], yf[:, j, 0:gw], start=(j == 0), stop=(j == 3)),
                             r=[yf_t[j], self.cst_t], w=[ps_t[4]])
                    for j in range(4):
                        P.op("pe", lambda e, j=j, gw=gw: e.matmul(ps[:, 5, 0:gw], self.onesm[:], ysq[:, j, 0:gw], start=(j == 0), stop=(j == 3)),
                             r=[yf_t[j], self.cst_t], w=[ps_t[5]])
                    P.op("dve", lambda e, gw=gw: e.tensor_tensor(out=st1[:, 0:gw], in0=ps[:, 4, 0:gw], in1=ps[:, 4, 0:gw], op=ALU.mult)
                         if False else e.tensor_copy(out=st1[:, 0:gw], in_=ps[:, 4, 0:gw]), r=[ps_t[4]], w=[st_t])
                    P.op("dve", lambda e, gw=gw: e.tensor_tensor(out=st2[:, 0:gw], in0=st1[:, 0:gw], in1=st1[:, 0:gw], op=ALU.mult),
                         r=[st_t], w=[st_t])
                    P.op("dve", lambda e, gw=gw: e.tensor_tensor(out=st2[:, 0:gw], in0=ps[:, 5, 0:gw], in1=st2[:, 0:gw], op=ALU.subtract),
                         r=[ps_t[5], st_t], w=[st_t])
                    P.op("act", lambda e, gw=gw: e.activation(out=st2[:, 0:gw], in_=st2[:, 0:gw], func=AF.Sqrt, bias=self.epsc[:, 0:1], scale=1.0),
                         r=[st_t, self.cst_t], w=[st_t])
                    P.op("dve", lambda e, gw=gw: e.reciprocal(out=st2[:, 0:gw], in_=st2[:, 0:gw]), r=[st_t], w=[st_t])
                    for j in range(4):
                        i = j % 2
                        P.op("dve", lambda e, j=j, i=i, gw=gw: e.tensor_tensor(out=t1[i][:, 0:gw], in0=yf[:, j, 0:gw], in1=st1[:, 0:gw], op=ALU.subtract),
                             r=[yf_t[j], st_t], w=[t1_t[i]])
                        P.op("dve", lambda e, i=i, gw=gw: e.tensor_tensor(out=t1[i][:, 0:gw], in0=t1[i][:, 0:gw], in1=st2[:, 0:gw], op=ALU.mult),
                             r=[t1_t[i], st_t], w=[t1_t[i]])
                        P.op("act", lambda e, j=j, i=i, gw=gw: e.activation(out=ycT[:, j, 0:gw], in_=t1[i][:, 0:gw], func=AF.Silu,
                                                                             scale=cvp[:, j, 32:33], bias=cvp[:, j, 33:34]),
                             r=[t1_t[i], cvp_t], w=[ycT_t])
                    for tt in range(gw // 128):
                        t = g * 4 + tt
                        for hh in range(2):
                            for j in range(4):
                                P.op("pe", lambda e, hh=hh, j=j, tt=tt: e.matmul(
                                    ps[:, 6 + hh, :], ycT[:, j, tt * 128:(tt + 1) * 128], woc[:, j, hh * 512:(hh + 1) * 512],
                                    start=(j == 0), stop=(j == 3)), r=[ycT_t, woc_t], w=[ps_t[6 + hh]])
                        cx = 1 if t >= 16 else 0
                        self.resid_add(t, 6, gbc[:, cx, :], gbc_t[cx], tmp, tmp_t)

        if "convonly" in self.debug:
            return
        a_aug = self.sb("a_aug", [33, 2304], BF16)
        a_t = Trk()
        wabd = self.sb("wabd", [33, 512], BF16)
        wabd_t = Trk()
        P.dma("pool", wabd[:], self.wabd_d, w=[wabd_t], key="cw0")
        ngb = self.sb("ngb", [128, 512])
        ngb_t = Trk()
        P.dma("sp", ngb[:], self.gla_norm_g_d.partition_broadcast(128), w=[ngb_t], key="gc")
        cln = self.sb("cln", [128, 2])
        P.op("dve", lambda e: e.memset(cln[:, 0:1], float(np.log(0.125))), w=[a_t])
        P.op("dve", lambda e: e.memset(cln[:, 1:2], 1.0), w=[a_t])
        P.op("dve", lambda e: e.memset(a_aug[32:33, :], 1.0), w=[a_t])
        with self.phase():
            waf = self.sb("waf", [128, 8, 32], BF16)
            waf_t = Trk()
            P.dma("pool", waf[:], win_v[:, :, OFF_AF:OFF_AF + 32], w=[waf_t], key="cw1")
            for g in range(5):
                gw = gwid[g]
                for kc in range(8):
                    P.op("pe", lambda e, kc=kc, g=g, gw=gw: e.matmul(ps[0:32, g % 2, 0:gw], waf[:, kc, :], hT[:, kc, g * 512:g * 512 + gw],
                                                                  start=(kc == 0), stop=(kc == 7)), r=[waf_t, hT_t[g]], w=[ps_t[g % 2]])
                P.op("act", lambda e, g=g, gw=gw: e.activation(out=a_aug[0:32, g * 512:g * 512 + gw], in_=ps[0:32, g % 2, 0:gw], func=AF.Copy),
                     r=[ps_t[g % 2]], w=[a_t])
        for hp in range(2):
            with self.phase():
                qr = self.sb("qr", [128, 18, 128], BF16)
                kr = self.sb("kr", [128, 18, 128], BF16)
                vv = self.sb("vv", [128, 18, 256], BF16)
                qkv_t = [Trk() for _ in range(18)]
                oacc = self.sb("oacc", [128, 18, 256])
                oacc_t = [Trk() for _ in range(18)]
                with self.phase():
                    wq = self.sb("wq0", [128, 8, 128], BF16)
                    wk = self.sb("wk0", [128, 8, 128], BF16)
                    wv = self.sb("wv0", [128, 8, 256], BF16)
                    w_t = Trk()
                    P.dma("pool", wq[:], win_v[:, :, OFF_Q + hp * 128:OFF_Q + (hp + 1) * 128], w=[w_t], key="cw0")
                    P.dma("pool", wk[:], win_v[:, :, OFF_K + hp * 128:OFF_K + (hp + 1) * 128], w=[w_t], key="cw0")
                    P.dma("pool", wv[:], win_v[:, :, OFF_V + hp * 256:OFF_V + (hp + 1) * 256], w=[w_t], key="cw0")
                    rC = self.sb("rC", [128, 18, 64])
                    rS = self.sb("rS", [128, 18, 64])
                    rope_t = Trk()
                    P.dma("sp", rC[:], self.ropeC_d, w=[rope_t], key="gc")
                    P.dma("act", rS[:], self.ropeS_d, w=[rope_t], key="gc2")
                    rt = [self.sb(f"rt{i}", [128, 128]) for i in range(2)]
                    rt2 = [self.sb(f"rt2{i}", [128, 128]) for i in range(2)]
                    rt_t = [Trk(), Trk()]
                    it = 0
                    for t in range(18):
                        for which in range(2):
                            i = it % 2
                            it += 1
                            bank = i
                            wsrc = wq if which == 0 else wk
                            for kc in range(8):
                                P.op("pe", lambda e, kc=kc, bank=bank, wsrc=wsrc, t=t: e.matmul(
                                    ps[:, bank, 0:128], hT[:, kc, t * 128:(t + 1) * 128], wsrc[:, kc, :], start=(kc == 0), stop=(kc == 7)),
                                    r=[w_t, hT_t[t // 4]], w=[ps_t[bank]])
                            x5 = ps[:, bank, 0:128].rearrange("p (h a w i) -> p h a w i", h=2, a=2, w=2, i=16)
                            S4 = rS[:, t, :].rearrange("p (a w i) -> p a w i", a=2, w=2, i=16)
                            tm5 = rt[i][:].rearrange("p (h a w i) -> p h a w i", h=2, a=2, w=2, i=16)
                            for w_ in range(2):
                                P.op("dve", lambda e, x5=x5, S4=S4, tm5=tm5, w_=w_: e.tensor_tensor(
                                    out=tm5[:, :, :, w_, :], in0=x5[:, :, :, 1 - w_, :],
                                    in1=S4[:, :, w_, :].unsqueeze(1).to_broadcast([128, 2, 2, 16]), op=ALU.mult),
                                    r=[ps_t[bank], rope_t], w=[rt_t[i]])
                            P.op("dve", lambda e, bank=bank, i=i, t=t: e.tensor_tensor(
                                out=rt2[i][:].rearrange("p (h d) -> p h d", h=2), in0=ps[:, bank, 0:128].rearrange("p (h d) -> p h d", h=2),
                                in1=rC[:, t, :].unsqueeze(1).to_broadcast([128, 2, 64]), op=ALU.mult),
                                r=[ps_t[bank], rope_t], w=[rt_t[i]])
                            dst = qr if which == 0 else kr
                            P.op("dve", lambda e, i=i, dst=dst, t=t: e.tensor_tensor(out=dst[:, t, :], in0=rt2[i][:], in1=rt[i][:], op=ALU.add),
                                 r=[rt_t[i]], w=[qkv_t[t]])
                        bank = 2 + t % 2
                        for kc in range(8):
                            P.op("pe", lambda e, kc=kc, bank=bank, t=t: e.matmul(
                                ps[:, bank, 0:256], hT[:, kc, t * 128:(t + 1) * 128], wv[:, kc, :], start=(kc == 0), stop=(kc == 7)),
                                r=[w_t, hT_t[t // 4]], w=[ps_t[bank]])
                        P.op("act", lambda e, bank=bank, t=t: e.activation(out=vv[:, t, :], in_=ps[:, bank, 0:256], func=AF.Copy),
                             r=[ps_t[bank]], w=[qkv_t[t]])
                with self.phase():
                    wgg = self.sb("wgg", [128, 8, 256], BF16)
                    wog = self.sb("wog", [128, 2, D], BF16)
                    wg_t = Trk()
                    P.dma("pool", wgg[:], win_v[:, :, OFF_G + hp * 256:OFF_G + (hp + 1) * 256], w=[wg_t], key="cw0")
                    P.dma("pool", wog[:], wout_v[:, hp * 2:hp * 2 + 2, :], w=[wg_t], key="cw0")
                    S = self.sb("S", [128, 128])
                    Sb = self.sb("Sb", [128, 128], BF16)
                    S_t = Trk()
                    nb = 2
                    e1 = [self.sb(f"e1_{i}", [128, 128]) for i in range(nb)]
                    la = [self.sb(f"la_{i}", [128, 128]) for i in range(nb)]
                    Eq = [self.sb(f"Eq_{i}", [128, 128]) for i in range(nb)]
                    Ek = [self.sb(f"Ek_{i}", [128, 128]) for i in range(nb)]
                    Ed = [self.sb(f"Ed_{i}", [128, 128]) for i in range(nb)]
                    ebl = [self.sb(f"ebl_{i}", [128, 2]) for i in range(nb)]
                    qt = [self.sb(f"qt_{i}", [128, 128], BF16) for i in range(nb)]
                    kt = [self.sb(f"kt_{i}", [128, 128], BF16) for i in range(nb)]
                    kd = [self.sb(f"kd_{i}", [128, 128], BF16) for i in range(nb)]
                    qtT = [self.sb(f"qtT_{i}", [128, 128], BF16) for i in range(nb)]
                    ktT = [self.sb(f"ktT_{i}", [128, 128], BF16) for i in range(nb)]
                    attm = [[self.sb(f"attm_{i}_{h}", [128, 128], BF16) for h in range(2)] for i in range(nb)]
                    tl_t = [Trk() for _ in range(nb)]
                    ex_t = [Trk() for _ in range(nb)]
                    qk_t = [Trk() for _ in range(nb)]
                    tr_t = [Trk() for _ in range(nb)]
                    at_t = [[Trk(), Trk()] for _ in range(nb)]
                    ssq = self.sb("ssq", [128, 4])
                    junk = self.sb("junk", [128, 128])
                    sgt = self.sb("sgt", [128, 256])
                    ot = self.sb("ot", [128, 256])
                    yg = self.sb("yg", [128, 256], BF16)
                    ygT = self.sb("ygT", [128, 2, 128], BF16)
                    po_t = Trk()
                    tix = 0
                    for d in range(2):
                        Mc, Ms, mk = (McF, MsF, mkF) if d == 0 else (McB, MsB, mkB)
                        zc0 = d * 256 + hp * 128
                        seqs = ([16, 17], list(range(16))) if d == 0 else ([17, 16], list(range(15, -1, -1)))
                        for si, seq in enumerate(seqs):
                            if si == 0:
                                P.op("dve", lambda e: e.memset(S[:], 0.0), w=[S_t])
                                P.op("dve", lambda e: e.memset(Sb[:], 0.0), w=[S_t])
                            for t in seq:
                                i = tix % nb
                                tix += 1
                                P.op("pe", lambda e, t=t: e.matmul(ps[:, 0, 0:128], a_aug[:, t * 128:(t + 1) * 128], wabd[:, zc0:zc0 + 128],
                                                                  start=True, stop=True), r=[a_t, wabd_t], w=[ps_t[0]])
                                P.op("act", lambda e, i=i: e.activation(out=e1[i][:], in_=ps[:, 0, 0:128], func=AF.Exp, scale=-1.0),
                                     r=[ps_t[0]], w=[tl_t[i]])
                                P.op("act", lambda e, i=i: e.activation(out=la[i][:], in_=e1[i][:], func=AF.Ln, bias=cln[:, 1:2], scale=1.0),
                                     r=[tl_t[i], a_t], w=[tl_t[i]])
                                P.op("pe", lambda e, i=i: e.matmul(ps[:, 0, 128:256], Mc, la[i][:], start=True, stop=True),
                                     r=[tl_t[i], gc_t], w=[ps_t[0]])
                                P.op("pe", lambda e, i=i: e.matmul(ps[:, 1, 0:128], Ms, la[i][:], start=True, stop=True),
                                     r=[tl_t[i], gc_t], w=[ps_t[1]])
                                P.op("pe", lambda e, i=i: e.matmul(ps[:, 2, 0:2], la[i][:], Ind, start=True, stop=True),
                                     r=[tl_t[i], gc_t], w=[ps_t[2]])
                                P.op("act", lambda e, i=i: e.activation(out=Eq[i][:], in_=ps[:, 0, 128:256], func=AF.Exp, bias=cln[:, 0:1], scale=1.0),
                                     r=[ps_t[0], a_t], w=[ex_t[i]])
                                P.op("act", lambda e, i=i: e.activation(out=Ek[i][:], in_=ps[:, 0, 128:256], func=AF.Exp, scale=-1.0),
                                     r=[ps_t[0]], w=[ex_t[i]])
                                P.op("act", lambda e, i=i: e.activation(out=Ed[i][:], in_=ps[:, 1, 0:128], func=AF.Exp),
                                     r=[ps_t[1]], w=[ex_t[i]])
                                P.op("act", lambda e, i=i: e.activation(out=ebl[i][:], in_=ps[:, 2, 0:2], func=AF.Exp),
                                     r=[ps_t[2]], w=[ex_t[i]])
                                P.op("dve", lambda e, i=i, t=t: e.tensor_tensor(out=qt[i][:], in0=qr[:, t, :], in1=Eq[i][:], op=ALU.mult),
                                     r=[qkv_t[t], ex_t[i]], w=[qk_t[i]])
                                P.op("dve", lambda e, i=i, t=t: e.tensor_tensor(out=kt[i][:], in0=kr[:, t, :], in1=Ek[i][:], op=ALU.mult),
                                     r=[qkv_t[t], ex_t[i]], w=[qk_t[i]])
                                P.op("dve", lambda e, i=i, t=t: e.tensor_tensor(out=kd[i][:], in0=kr[:, t, :], in1=Ed[i][:], op=ALU.mult),
                                     r=[qkv_t[t], ex_t[i]], w=[qk_t[i]])
                                P.op("pe", lambda e, i=i: e.transpose(psb[:, 2, 128:256], qt[i][:], self.identb[:]),
                                     r=[qk_t[i], self.cst_t], w=[ps_t[2]])
                                P.op("pe", lambda e, i=i: e.transpose(psb[:, 2, 256:384], kt[i][:], self.identb[:]),
                                     r=[qk_t[i], self.cst_t], w=[ps_t[2]])
                                P.op("act", lambda e, i=i: e.activation(out=qtT[i][:], in_=psb[:, 2, 128:256], func=AF.Copy),
                                     r=[ps_t[2]], w=[tr_t[i]])
                                P.op("act", lambda e, i=i: e.activation(out=ktT[i][:], in_=psb[:, 2, 256:384], func=AF.Copy),
                                     r=[ps_t[2]], w=[tr_t[i]])
                                for h in range(2):
                                    hr = slice(h * 64, (h + 1) * 64)
                                    P.op("pe", lambda e, i=i, h=h, hr=hr: e.matmul(ps[:, 3 + h, 0:128], ktT[i][hr, :], qtT[i][hr, :], start=True, stop=True),
                                         r=[tr_t[i]], w=[ps_t[3 + h]])
                                    P.op("dve", lambda e, i=i, h=h: e.tensor_tensor(out=attm[i][h][:], in0=ps[:, 3 + h, 0:128], in1=mk, op=ALU.mult),
                                         r=[ps_t[3 + h], gc_t], w=[at_t[i][h]])
                                for c in ((0, 1) if d == 0 else (1, 0)):
                                    cr = slice(c * 64, (c + 1) * 64)
                                    for h in range(2):
                                        hr = slice(h * 64, (h + 1) * 64)
                                        P.op("pe", lambda e, i=i, h=h, cr=cr, t=t: e.matmul(
                                            ps[cr, 5, h * 128:(h + 1) * 128], attm[i][h][:, cr], vv[:, t, h * 128:(h + 1) * 128], start=True, stop=False),
                                            r=[at_t[i][h], qkv_t[t]], w=[ps_t[5]])
                                        P.op("pe", lambda e, i=i, h=h, cr=cr, hr=hr: e.matmul(
                                            ps[cr, 5, h * 128:(h + 1) * 128], qtT[i][hr, cr], Sb[hr, :], start=False, stop=True),
                                            r=[tr_t[i], S_t], w=[ps_t[5]])
                                    P.op("pe", lambda e, i=i, cr=cr, t=t: e.matmul(ps[:, 6, 0:256], kd[i][cr, :], vv[cr, t, :], start=True, stop=True),
                                         r=[qk_t[i], qkv_t[t]], w=[ps_t[6]])
                                    for h in range(2):
                                        hr = slice(h * 64, (h + 1) * 64)
                                        P.op("dve", lambda e, i=i, h=h, hr=hr, c=c: e.scalar_tensor_tensor(
                                            out=S[hr, :], in0=S[hr, :], scalar=ebl[i][hr, c:c + 1], in1=ps[hr, 6, h * 128:(h + 1) * 128],
                                            op0=ALU.mult, op1=ALU.add), r=[S_t, ex_t[i], ps_t[6]], w=[S_t])
                                    P.op("act", lambda e: e.activation(out=Sb[:], in_=S[:], func=AF.Copy), r=[S_t], w=[S_t])
                                if d == 0:
                                    P.op("act", lambda e, t=t: e.activation(out=oacc[:, t, :], in_=ps[:, 5, 0:256], func=AF.Copy),
                                         r=[ps_t[5]], w=[oacc_t[t]])
                                    continue
                                P.op("dve", lambda e, t=t: e.tensor_tensor(out=ot[:], in0=ps[:, 5, 0:256], in1=oacc[:, t, :], op=ALU.add),
                                     r=[ps_t[5], oacc_t[t]], w=[po_t])
                                for h in range(2):
                                    P.op("act", lambda e, h=h: e.activation(out=junk[:], in_=ot[:, h * 128:(h + 1) * 128], func=AF.Square,
                                                                           accum_out=ssq[:, h:h + 1]), r=[po_t], w=[po_t])
                                P.op("act", lambda e: e.activation(out=ssq[:, 2:4], in_=ssq[:, 0:2], func=AF.Sqrt, scale=1.0 / 128, bias=self.epsc[:, 0:1]),
                                     r=[po_t, self.cst_t], w=[po_t])
                                P.op("dve", lambda e: e.reciprocal(out=ssq[:, 2:4], in_=ssq[:, 2:4]), r=[po_t], w=[po_t])
                                for kc in range(8):
                                    P.op("pe", lambda e, kc=kc, t=t: e.matmul(ps[:, 7, 0:256], hT[:, kc, t * 128:(t + 1) * 128], wgg[:, kc, :],
                                                                           start=(kc == 0), stop=(kc == 7)), r=[wg_t, hT_t[t // 4]], w=[ps_t[7]])
                                P.op("act", lambda e: e.activation(out=sgt[:], in_=ps[:, 7, 0:256], func=AF.Silu), r=[ps_t[7]], w=[po_t])
                                for h in range(2):
                                    P.op("dve", lambda e, h=h: e.tensor_scalar(out=ot[:, h * 128:(h + 1) * 128], in0=ot[:, h * 128:(h + 1) * 128],
                                                                               scalar1=ssq[:, 2 + h:3 + h], scalar2=None, op0=ALU.mult),
                                         r=[po_t], w=[po_t])
                                P.op("dve", lambda e: e.tensor_tensor(out=ot[:], in0=ot[:], in1=ngb[:, hp * 256:(hp + 1) * 256], op=ALU.mult),
                                     r=[po_t, ngb_t], w=[po_t])
                                P.op("dve", lambda e: e.tensor_tensor(out=yg[:], in0=ot[:], in1=sgt[:], op=ALU.mult), r=[po_t], w=[po_t])
                                for h in range(2):
                                    P.op("pe", lambda e, h=h: e.transpose(psb[:, 2, 512 + h * 128:640 + h * 128], yg[:, h * 128:(h + 1) * 128], self.identb[:]),
                                         r=[po_t, self.cst_t], w=[ps_t[2]])
                                P.op("act", lambda e: e.activation(out=ygT[:].rearrange("p h t -> p (h t)"), in_=psb[:, 2, 512:768], func=AF.Copy),
                                     r=[ps_t[2]], w=[po_t])
                                for hh in range(2):
                                    for h in range(2):
                                        P.op("pe", lambda e, hh=hh, h=h: e.matmul(ps[:, 6 + hh, :], ygT[:, h, :], wog[:, h, hh * 512:(hh + 1) * 512],
                                                                               start=(h == 0), stop=(h == 1)), r=[po_t, wg_t], w=[ps_t[6 + hh]])
                                cx = 1 if t >= 16 else 0
                                self.resid_add(t, 6, gbc[:, cx, :], gbc_t[cx], tmp, tmp_t)

    def sample(self, b):
        nc, P = self.nc, self.P
        xs, xs_t = self.xs, self.xs_t
        for t in range(16):
            q = "sp" if t % 2 == 0 else "act"
            P.dma(q, xs[:, t, :], self.x_d[b, t * 128:(t + 1) * 128, :], w=[xs_t[t]], key=f"x{t}")
        for t in range(2):
            P.dma("sp", xs[:, 16 + t, :], self.ctx_d[b, t * 128:(t + 1) * 128, :], w=[xs_t[16 + t]], key=f"x{16 + t}")
        if "glatest" in self.debug:
            with self.phase():
                self.alloc_hT()
                with self.phase():
                    self.prenorm(b, 0, 0, list(range(18)))
                self.mixer_gla(b, 0)
            if b == 0:
                for t in range(18):
                    P.dma("sp", self.dbg_xs[:, t, :], xs[:, t, :], r=[xs_t[t]], w=[Trk()], key="dbg")
            return
        if "natest" in self.debug:
            with self.phase():
                self.alloc_hT()
                with self.phase():
                    self.prenorm(b, 1, 0, list(range(18)))
                self.mixer_na(b, 1)
            if b == 0:
                for t in range(18):
                    P.dma("sp", self.dbg_xs[:, t, :], xs[:, t, :], r=[xs_t[t]], w=[Trk()], key="dbg")
            return
        if "moetest" in self.debug:
            with self.phase():
                self.moe2(b, 0, 18)
            if b == 0:
                for t in range(18):
                    P.dma("sp", self.dbg_xs[:, t, :], xs[:, t, :], r=[xs_t[t]], w=[Trk()], key="dbg")
            return
        with self.phase():
            self.alloc_hT()
            with self.phase():
                self.prenorm(b, 0, 0, list(range(18)))
            self.mixer_gla(b, 0)
        with self.phase():
            self.moe2(b, 0, 18)
        with self.phase():
            self.alloc_hT()
            with self.phase():
                self.prenorm(b, 1, 0, list(range(18)))
            self.mixer_na(b, 1)
        with self.phase():
            self.moe2(b, 1, 16)
        with self.phase():
            fng = self.sb("fng_sb", [128, D])
            fng_t = Trk()
            P.dma("sp", fng[:], self.fng_d.partition_broadcast(128), w=[fng_t], key="gc")
            junk = self.sb("fjunk", [128, D], BF16)
            junk_t = Trk()
            fss = [self.sb(f"fss{i}", [128, 2]) for i in range(2)]
            fss_t = [Trk(), Trk()]
            for t in range(16):
                i = t % 2
                P.op("act", lambda e, t=t, i=i: e.activation(out=junk[:], in_=xs[:, t, :], func=AF.Square, accum_out=fss[i][:, 0:1]),
                     r=[xs_t[t]], w=[junk_t, fss_t[i]])
                P.op("act", lambda e, i=i: e.activation(out=fss[i][:, 1:2], in_=fss[i][:, 0:1], func=AF.Sqrt, scale=1.0 / D,
                                                        bias=self.epsc[:, 0:1]), r=[fss_t[i], self.cst_t], w=[fss_t[i]])
                P.op("dve", lambda e, i=i: e.reciprocal(out=fss[i][:, 1:2], in_=fss[i][:, 1:2]), r=[fss_t[i]], w=[fss_t[i]])
                P.op("dve", lambda e, t=t, i=i: e.scalar_tensor_tensor(out=xs[:, t, :], in0=xs[:, t, :], scalar=fss[i][:, 1:2],
                                                                       in1=fng[:], op0=ALU.mult, op1=ALU.mult),
                     r=[xs_t[t], fss_t[i], fng_t], w=[xs_t[t]])
                ot = Trk()
                P.dma("sp" if t % 2 == 0 else "act", self.out_d[b, t * 128:(t + 1) * 128, :], xs[:, t, :], r=[xs_t[t]], w=[ot],
                      key=f"o{t}")
                self.out_trks.append(ot)
        return
        for t in range(16):
            ot = Trk()
            P.dma("sp", self.out_d[b, t * 128:(t + 1) * 128, :], xs[:, t, :], r=[xs_t[t]], w=[ot], key=f"o{t % 4}")
            self.out_trks.append(ot)


_BT_CACHE = {}


def gla_consts():
    s_ = np.arange(128)[:, None]
    t_ = np.arange(128)[None, :]
    same = (s_ // 64) == (t_ // 64)
    c = np.zeros((128, 772), np.float32)
    c[:, 0:128] = np.where(same & (s_ <= t_), -1.0 / 16, 0.0)
    c[:, 128:256] = np.where(same & (s_ >= t_), -1.0 / 16, 0.0)
    c[:, 256:384] = np.where(same & (s_ > t_), -1.0 / 16, 0.0)
    c[:, 384:512] = np.where(same & (s_ < t_), -1.0 / 16, 0.0)
    c[:, 512:640] = np.where(same & (s_ <= t_), 1.0, 0.0)
    c[:, 640:768] = np.where(same & (s_ >= t_), 1.0, 0.0)
    c[0:64, 768] = -1.0 / 16
    c[64:128, 769] = -1.0 / 16
    return c


def rope_tables():
    tok = np.arange(SEQ)
    row = (tok // 64).astype(np.float32)
    col = (tok % 64).astype(np.float32)
    half = 16
    inv = (10000.0 ** (-np.arange(half, dtype=np.float32) / half)).astype(np.float32)
    C = np.ones((2304, 64), np.float32)
    S = np.zeros((2304, 64), np.float32)
    for a, pos in enumerate((row, col)):
        ang = pos[:, None] * inv[None, :]
        cs, sn = np.cos(ang).astype(np.float32), np.sin(ang).astype(np.float32)
        C[:SEQ, a * 32:a * 32 + 16] = cs
        C[:SEQ, a * 32 + 16:a * 32 + 32] = cs
        S[:SEQ, a * 32:a * 32 + 16] = -sn
        S[:SEQ, a * 32 + 16:a * 32 + 32] = sn
    f = lambda z: np.ascontiguousarray(z.reshape(18, 128, 64).transpose(1, 0, 2))
    return f(C), f(S)


def na_bias_table(rpb):
    key = rpb.tobytes()
    if key in _BT_CACHE:
        return _BT_CACHE[key]
    rpb = np.asarray(rpb, dtype=np.float32)
    kk = np.arange(128)
    krl, kc = kk // 64, kk % 64
    qq = np.arange(128)
    qrl, qc = qq // 64, qq % 64
    out = np.empty((16, 128, 5, 5, 128), np.float32)
    for vi, p in enumerate((0, 1, 2, 14, 15)):
        Bp = min(max(2 * p - 4, 0), 22)
        qrow = 2 * p + qrl
        rs = np.clip(qrow - 4, 0, 24)
        cs = np.clip(qc - 8, 0, 48)
        for j in range(5):
            krow = Bp + 2 * j + krl
            valid = ((krow[:, None] >= rs[None, :]) & (krow[:, None] < rs[None, :] + 8)
                     & (kc[:, None] >= cs[None, :]) & (kc[:, None] < cs[None, :] + 16))
            dr = np.clip(krow[:, None] - qrow[None, :] + 7, 0, 14)
            dc = np.clip(kc[:, None] - qc[None, :] + 15, 0, 30)
            vals = rpb[:, dr, dc]
            out[:, :, vi, j, :] = np.where(valid[None], vals, np.float32(-30000.0))
    res = np.ascontiguousarray(out.reshape(16, 128, 5, 640))
    _BT_CACHE[key] = res
    return res


def host_layouts(inp, core):
    b0 = core * NS
    f = lambda a: np.ascontiguousarray(a, dtype=np.float32)
    m = {}
    m["x"] = f(inp["x"][b0:b0 + NS])
    m["ctx"] = f(inp["ctx"][b0:b0 + NS])
    c5 = np.concatenate([inp["c"][b0:b0 + NS], inp["c_ctx"][None, :]], axis=0)
    m["cT"] = f(c5.T.reshape(8, 128, 5).transpose(1, 0, 2))
    m["ada_w"] = f(inp["ada_w"])
    m["ada_bT"] = f(inp["ada_b"].reshape(2, 48, 128).transpose(0, 2, 1))
    m["n1gT"] = f(inp["norm1_g"].reshape(2, 8, 128).transpose(0, 2, 1))
    m["n2gT"] = f(inp["norm2_g"].reshape(2, 8, 128).transpose(0, 2, 1))
    m["fng"] = f(inp["final_norm_g"])
    m["ident"] = np.eye(128, dtype=np.float32)
    m["na_w_qkv"] = f(inp["na_w_qkv"][0])
    m["gla_w_in"] = f(inp["gla_conv_w_in"][0])
    m["gla_w_out"] = f(inp["gla_conv_w_out"][0])
    m["gconst"] = gla_consts()
    cvp = np.zeros((128, 4, 34), np.float32)
    cvp[:, :, 0:31] = inp["conv_dw_w"][0].reshape(31, 4, 128).transpose(2, 1, 0)
    cvp[:, :, 31] = inp["conv_dw_b"][0].reshape(4, 128).T
    cvp[:, :, 32] = inp["conv_ln_g"][0].reshape(4, 128).T
    cvp[:, :, 33] = inp["conv_ln_b"][0].reshape(4, 128).T
    m["convp"] = cvp
    wabd = np.zeros((33, 512), np.float32)
    wabd[0:16, 0:256] = inp["gla_wa_fwd"][0]
    wabd[16:32, 256:512] = inp["gla_wa_bwd"][0]
    wabd[32, 0:256] = inp["gla_ba_fwd"][0]
    wabd[32, 256:512] = inp["gla_ba_bwd"][0]
    m["wabd"] = wabd
    m["gla_norm_g"] = f(inp["gla_norm_g"][0])
    rc, rs_ = rope_tables()
    m["ropeC"] = rc
    m["ropeS"] = rs_
    m["na_w_out"] = f(inp["na_w_out"][0])
    m["na_bt"] = na_bias_table(inp["na_rpb"][0])
    mcst = np.zeros((128, 192), np.float32)
    mcst[:, 0:128] = (np.arange(128)[:, None] < np.arange(128)[None, :])
    mcst[:, 128:160] = np.arange(NE)[None, :] * 2560.0
    mcst[:, 160:192] = np.arange(NE)[None, :]
    m["mconst"] = mcst
    m["router_w"] = f(inp["router_w"])
    m["router_b"] = f(inp["router_b"])
    m["bguT"] = f(inp["expert_b_gu"].reshape(2, NE, 16, 128).transpose(0, 3, 1, 2))
    m["expert_b_down"] = f(inp["expert_b_down"])
    m["expert_w_gu"] = f(inp["expert_w_gu"])
    m["expert_w_down"] = f(inp["expert_w_down"])
    return m


_CACHE = {}


def kernel(**inputs):
    inp = {k: np.asarray(v) for k, v in inputs.items()}
    if "nc" not in _CACHE:
        bld = Builder()
        _CACHE["nc"] = bld.build()
    nc = _CACHE["nc"]
    in_maps = [host_layouts(inp, c) for c in range(NCORES)]
    res = run_bass_kernel_spmd(nc, in_maps, core_ids=list(range(NCORES)))
    out = np.concatenate([r["out"] for r in res.results], axis=0)
    return out.astype(np.float32)
```
